# Optimizing a Trainium2 kernel written in Bass

```python
import jax
import jax.numpy as jnp
from jax import lax
import numpy as np

D_MODEL = 1024
BATCH = 8
SEQ = 2048
DEPTH = 1

CTX_LEN = 256
GRID_W = 64
EPS = 1e-6
ATT_HEADS = 8
ATT_KV_HEADS = 2
ATT_GROUP = ATT_HEADS // ATT_KV_HEADS
HEAD_DIM = 64
WINDOW = 128
ATT_BLOCK = 128
ROPE_BASE = 10000.0
AXIS_ROT = HEAD_DIM // 2
GLA_HEADS = 4
GLA_DK = 64
GLA_DV = 128
GLA_RANK = 16
GLA_TAU = 16.0
GLA_CHUNK = 64
N_EXPERTS = 32
TOP_K = 4
D_FF = D_MODEL
SWIGLU_ALPHA = 1.702
SWIGLU_LIMIT = 7.0
MOE_BLOCK = 256

ATT_W = ATT_HEADS * HEAD_DIM
ATT_KV_W = ATT_KV_HEADS * HEAD_DIM
GLA_K_W = GLA_HEADS * GLA_DK
GLA_V_W = GLA_HEADS * GLA_DV
IN_SPLITS = (ATT_W, ATT_KV_W, ATT_KV_W, GLA_K_W, GLA_K_W, GLA_V_W, GLA_V_W, GLA_RANK, GLA_RANK, D_MODEL, D_MODEL)
C_IN = sum(IN_SPLITS)

kernel_name = 'hybrid_swa_gla_moe_dit_layer'


def rms_norm(x, g):
    xf = x.astype(jnp.float32)
    y = xf * lax.rsqrt(jnp.mean(xf * xf, axis=-1, keepdims=True) + EPS)
    return (y * g.astype(jnp.float32)).astype(x.dtype)


def axial_rope_angles(n_tok):
    rows = n_tok // GRID_W
    row = jnp.repeat(jnp.arange(rows, dtype=jnp.float32), GRID_W)
    col = jnp.tile(jnp.arange(GRID_W, dtype=jnp.float32), rows)
    inv = ROPE_BASE ** (-jnp.arange(0, AXIS_ROT, 2, dtype=jnp.float32) / AXIS_ROT)
    return row[:, None] * inv, col[:, None] * inv


def rotate_pairs(x, ang):
    m = x.shape[-1] // 2
    cos = jnp.cos(ang)[:, None, :].astype(x.dtype)
    sin = jnp.sin(ang)[:, None, :].astype(x.dtype)
    x1, x2 = x[..., :m], x[..., m:]
    return jnp.concatenate([x1 * cos - x2 * sin, x2 * cos + x1 * sin], axis=-1)


def axial_rope(x, ang_r, ang_c):
    return jnp.concatenate([rotate_pairs(x[..., :AXIS_ROT], ang_r),
                            rotate_pairs(x[..., AXIS_ROT:], ang_c)], axis=-1)


def mixer_inputs(h, w_in, q_norm, k_norm, w_alpha_f, b_alpha_f, w_alpha_b, b_alpha_b):
    B, T, _ = h.shape
    split_at = np.cumsum(IN_SPLITS)[:-1].tolist()
    aq, ak, av, gq, gk, gv, gr, lr_f, lr_b, ga, gg = jnp.split(h @ w_in, split_at, axis=-1)
    aq = rms_norm(aq.reshape(B, T, ATT_HEADS, HEAD_DIM), q_norm)
    ak = rms_norm(ak.reshape(B, T, ATT_KV_HEADS, HEAD_DIM), k_norm)
    av = av.reshape(B, T, ATT_KV_HEADS, HEAD_DIM)
    gq = gq.reshape(B, T, GLA_HEADS, GLA_DK) * (GLA_DK ** -0.5)
    gk = gk.reshape(B, T, GLA_HEADS, GLA_DK)
    gv = gv.reshape(B, T, GLA_HEADS, GLA_DV)
    la_f = jax.nn.log_sigmoid((lr_f @ w_alpha_f + b_alpha_f).astype(jnp.float32)) / GLA_TAU
    la_b = jax.nn.log_sigmoid((lr_b @ w_alpha_b + b_alpha_b).astype(jnp.float32)) / GLA_TAU
    la_f = la_f.reshape(B, T, GLA_HEADS, GLA_DK)
    la_b = la_b.reshape(B, T, GLA_HEADS, GLA_DK)
    return (aq, ak, av, gq, gk, gv, gr, la_f, la_b, ga, gg)


def windowed_attention(q, k, v, kc, vc, sink):
    B, T = q.shape[:2]
    Lc = kc.shape[1]
    nb = T // ATT_BLOCK
    nl = 3 * ATT_BLOCK
    scale = HEAD_DIM ** -0.5
    qb = q.reshape(B, nb, ATT_BLOCK, ATT_KV_HEADS, ATT_GROUP, HEAD_DIM)
    pad = ((0, 0), (ATT_BLOCK, ATT_BLOCK), (0, 0), (0, 0))
    kp = jnp.pad(k, pad).reshape(B, nb + 2, ATT_BLOCK, ATT_KV_HEADS, HEAD_DIM)
    vp = jnp.pad(v, pad).reshape(B, nb + 2, ATT_BLOCK, ATT_KV_HEADS, HEAD_DIM)
    kw = jnp.concatenate([kp[:, :-2], kp[:, 1:-1], kp[:, 2:]], axis=2)
    vw = jnp.concatenate([vp[:, :-2], vp[:, 1:-1], vp[:, 2:]], axis=2)
    qi = jnp.arange(ATT_BLOCK)[:, None]
    kj = jnp.arange(nl)[None, :]
    rel = kj - ATT_BLOCK - qi
    kpos = jnp.arange(nb)[:, None, None] * ATT_BLOCK - ATT_BLOCK + kj[None]
    mask = (jnp.abs(rel) <= WINDOW)[None] & (kpos >= 0) & (kpos < T)
    s_loc = jnp.einsum('bnqkgd,bnskd->bnkgqs', qb, kw).astype(jnp.float32) * scale
    s_loc = jnp.where(mask[None, :, None, None], s_loc, -jnp.inf)
    s_ctx = jnp.einsum('bnqkgd,bckd->bnkgqc', qb, kc).astype(jnp.float32) * scale
    s_sink = jnp.broadcast_to(sink.reshape(ATT_KV_HEADS, ATT_GROUP)[None, None, :, :, None, None].astype(jnp.float32),
                              s_loc.shape[:-1] + (1,))
    p = jax.nn.softmax(jnp.concatenate([s_loc, s_ctx, s_sink], axis=-1), axis=-1).astype(q.dtype)
    o = (jnp.einsum('bnkgqs,bnskd->bnqkgd', p[..., :nl], vw)
         + jnp.einsum('bnkgqc,bckd->bnqkgd', p[..., nl:nl + Lc], vc))
    return o.reshape(B, T, ATT_W)


def context_attention(qc, kc, vc, sink):
    B, Lc = qc.shape[:2]
    qg = qc.reshape(B, Lc, ATT_KV_HEADS, ATT_GROUP, HEAD_DIM)
    s = jnp.einsum('bqkgd,bckd->bkgqc', qg, kc).astype(jnp.float32) * (HEAD_DIM ** -0.5)
    s_sink = jnp.broadcast_to(sink.reshape(ATT_KV_HEADS, ATT_GROUP)[None, :, :, None, None].astype(jnp.float32),
                              s.shape[:-1] + (1,))
    p = jax.nn.softmax(jnp.concatenate([s, s_sink], axis=-1), axis=-1)[..., :Lc].astype(qc.dtype)
    return jnp.einsum('bkgqc,bckd->bqkgd', p, vc).reshape(B, Lc, ATT_W)


def gla_chunk_scan(q, k, v, log_a, s0):
    B, T, H, dk = q.shape
    dv = v.shape[-1]
    nc = T // GLA_CHUNK

    def chunks(a):
        return a.reshape(B, nc, GLA_CHUNK, H, a.shape[-1]).transpose(1, 0, 3, 2, 4)

    causal = jnp.tril(jnp.ones((GLA_CHUNK, GLA_CHUNK), bool))

    def step(S, inp):
        qc, kc, vc, gc = inp
        b = jnp.cumsum(gc.astype(jnp.float32), axis=2)
        o_inter = jnp.einsum('bhcd,bhde->bhce', qc * jnp.exp(b), S)
        decay = jnp.exp(jnp.where(causal[:, :, None], b[:, :, :, None, :] - b[:, :, None, :, :], -jnp.inf))
        a = jnp.einsum('bhid,bhjd,bhijd->bhij', qc, kc, decay)
        o = o_inter + jnp.einsum('bhij,bhje->bhie', a, vc)
        b_last = b[:, :, -1:, :]
        S = jnp.exp(b_last[:, :, 0, :])[..., None] * S + jnp.einsum('bhjd,bhje->bhde', kc * jnp.exp(b_last - b), vc)
        return S, o

    S, o = lax.scan(step, s0, (chunks(q), chunks(k), chunks(v), chunks(log_a)))
    o = o.transpose(1, 0, 3, 2, 4).reshape(B, T, H, dv)
    return o.astype(v.dtype), S


def bidirectional_gla(q, k, v, la_f, la_b, qc, kc, vc, la_fc, la_bc):
    B = q.shape[0]
    s0 = jnp.zeros((B, GLA_HEADS, GLA_DK, GLA_DV), jnp.float32)
    fl = lambda a: jnp.flip(a, axis=1)
    oc_f, s_f = gla_chunk_scan(qc, kc, vc, la_fc, s0)
    oc_b, s_b = gla_chunk_scan(fl(qc), fl(kc), fl(vc), fl(la_bc), s0)
    o_f, _ = gla_chunk_scan(q, k, v, la_f, s_f)
    o_b, _ = gla_chunk_scan(fl(q), fl(k), fl(v), fl(la_b), s_b)
    return o_f + fl(o_b), oc_f + fl(oc_b)


def merge_branches(attn_o, gla_o, gr, ga, gg, gla_norm, w_branch_attn, w_branch_gla, w_out):
    B, T = gr.shape[:2]
    o = rms_norm(gla_o, gla_norm) * jax.nn.silu(gr.reshape(B, T, GLA_HEADS, GLA_DV))
    y = (jax.nn.sigmoid(ga) * (attn_o @ w_branch_attn)
         + jax.nn.sigmoid(gg) * (o.reshape(B, T, GLA_V_W) @ w_branch_gla))
    return y @ w_out


def moe_ffn(h, w_router, b_router, w1, b1, w2, b2):
    shp = h.shape
    xt = h.reshape(-1, D_MODEL)
    n = xt.shape[0]
    logits = (xt @ w_router + b_router).astype(jnp.float32)
    top_v, top_i = lax.top_k(logits, TOP_K)
    wts = jax.nn.softmax(top_v, axis=-1).astype(h.dtype)
    e_flat = top_i.reshape(-1)
    order = jnp.argsort(e_flat)
    e_sorted = e_flat[order]
    tok_sorted = order // TOP_K
    w_sorted = wts.reshape(-1)[order]
    counts = jnp.bincount(e_flat, length=N_EXPERTS)
    padded = (counts + MOE_BLOCK - 1) // MOE_BLOCK * MOE_BLOCK
    pad_end = jnp.cumsum(padded)
    pad_start = pad_end - padded
    start = jnp.cumsum(counts) - counts
    dest = pad_start[e_sorted] + jnp.arange(n * TOP_K) - start[e_sorted]
    cap = (n * TOP_K + N_EXPERTS * (MOE_BLOCK - 1)) // MOE_BLOCK * MOE_BLOCK
    n_blk = cap // MOE_BLOCK
    buf_tok = jnp.zeros((cap,), jnp.int32).at[dest].set(tok_sorted.astype(jnp.int32))
    blk_e = jnp.minimum(jnp.searchsorted(pad_end, jnp.arange(n_blk) * MOE_BLOCK, side='right'), N_EXPERTS - 1)
    xs = xt[buf_tok].reshape(n_blk, MOE_BLOCK, D_MODEL)

    def expert_block(args):
        xb, e = args
        gu = xb @ w1[e] + b1[e]
        gate = jnp.minimum(gu[:, :D_FF], SWIGLU_LIMIT)
        up = jnp.clip(gu[:, D_FF:], -SWIGLU_LIMIT, SWIGLU_LIMIT)
        act = gate * jax.nn.sigmoid(SWIGLU_ALPHA * gate) * (up + 1)
        return act @ w2[e] + b2[e]

    ys = lax.map(expert_block, (xs, blk_e)).reshape(cap, D_MODEL)
    y = jnp.zeros_like(xt).at[tok_sorted].add(ys[dest] * w_sorted[:, None])
    return y.reshape(shp)


def setup_inputs(seed: int = 0) -> dict:
    key = jax.random.key(seed)
    ks = jax.random.split(key, 26)
    D = D_MODEL

    def nrm(k, shape, scale):
        return jax.random.normal(k, shape, jnp.float32) * scale

    return {
        'x': nrm(ks[0], (BATCH, SEQ, D), 1.0),
        'c': nrm(ks[1], (BATCH, D), 1.0),
        'ctx': nrm(ks[2], (BATCH, CTX_LEN, D), 1.0),
        'c_ctx': nrm(ks[3], (D,), 1.0),
        'w_mod': nrm(ks[4], (DEPTH, D, 6 * D), 0.5 * D ** -0.5),
        'b_mod': nrm(ks[5], (DEPTH, 6 * D), 0.02),
        'norm1': 1.0 + nrm(ks[6], (DEPTH, D), 0.02),
        'norm2': 1.0 + nrm(ks[7], (DEPTH, D), 0.02),
        'w_in': nrm(ks[8], (DEPTH, D, C_IN), D ** -0.5),
        'q_norm': 1.0 + nrm(ks[9], (DEPTH, HEAD_DIM), 0.02),
        'k_norm': 1.0 + nrm(ks[10], (DEPTH, HEAD_DIM), 0.02),
        'attn_sink': nrm(ks[11], (DEPTH, ATT_HEADS), 0.5),
        'w_alpha_f': nrm(ks[12], (DEPTH, GLA_RANK, GLA_K_W), GLA_RANK ** -0.5),
        'b_alpha_f': nrm(ks[13], (DEPTH, GLA_K_W), 0.1),
        'w_alpha_b': nrm(ks[14], (DEPTH, GLA_RANK, GLA_K_W), GLA_RANK ** -0.5),
        'b_alpha_b': nrm(ks[15], (DEPTH, GLA_K_W), 0.1),
        'gla_norm': 1.0 + nrm(ks[16], (DEPTH, GLA_DV), 0.02),
        'w_branch_attn': nrm(ks[17], (DEPTH, ATT_W, D), ATT_W ** -0.5),
        'w_branch_gla': nrm(ks[18], (DEPTH, GLA_V_W, D), GLA_V_W ** -0.5),
        'w_out': nrm(ks[19], (DEPTH, D, D), D ** -0.5),
        'w_router': nrm(ks[20], (DEPTH, D, N_EXPERTS), D ** -0.5),
        'b_router': nrm(ks[21], (DEPTH, N_EXPERTS), 0.01),
        'w_exp_in': nrm(ks[22], (DEPTH, N_EXPERTS, D, 2 * D_FF), D ** -0.5),
        'b_exp_in': nrm(ks[23], (DEPTH, N_EXPERTS, 2 * D_FF), 0.02),
        'w_exp_out': nrm(ks[24], (DEPTH, N_EXPERTS, D_FF, D), D_FF ** -0.5),
        'b_exp_out': nrm(ks[25], (DEPTH, N_EXPERTS, D), 0.02),
    }


def reference(x, c, ctx, c_ctx, w_mod, b_mod, norm1, norm2, w_in, q_norm, k_norm, attn_sink,
              w_alpha_f, b_alpha_f, w_alpha_b, b_alpha_b, gla_norm, w_branch_attn, w_branch_gla,
              w_out, w_router, b_router, w_exp_in, b_exp_in, w_exp_out, b_exp_out):
    ang_r, ang_c = axial_rope_angles(x.shape[1])
    for l in range(DEPTH):
        mod = (jax.nn.silu(c) @ w_mod[l] + b_mod[l])[:, None, :]
        mod_c = jax.nn.silu(c_ctx) @ w_mod[l] + b_mod[l]
        sh1, sc1, gt1, sh2, sc2, gt2 = jnp.split(mod, 6, axis=-1)
        csh1, csc1, cgt1, csh2, csc2, cgt2 = jnp.split(mod_c, 6, axis=-1)
        h = rms_norm(x, norm1[l]) * (1 + sc1) + sh1
        hc = rms_norm(ctx, norm1[l]) * (1 + csc1) + csh1
        proj_w = (w_in[l], q_norm[l], k_norm[l], w_alpha_f[l], b_alpha_f[l], w_alpha_b[l], b_alpha_b[l])
        aq, ak, av, gq, gk, gv, gr, laf, lab, ga, gg = mixer_inputs(h, *proj_w)
        caq, cak, cav, cgq, cgk, cgv, cgr, claf, clab, cga, cgg = mixer_inputs(hc, *proj_w)
        aq = axial_rope(aq, ang_r, ang_c)
        ak = axial_rope(ak, ang_r, ang_c)
        attn_o = windowed_attention(aq, ak, av, cak, cav, attn_sink[l])
        gla_o, gla_oc = bidirectional_gla(gq, gk, gv, laf, lab, cgq, cgk, cgv, claf, clab)
        merge_w = (gla_norm[l], w_branch_attn[l], w_branch_gla[l], w_out[l])
        moe_w = (w_router[l], b_router[l], w_exp_in[l], b_exp_in[l], w_exp_out[l], b_exp_out[l])
        x_next = x + gt1 * merge_branches(attn_o, gla_o, gr, ga, gg, *merge_w)
        h2 = rms_norm(x_next, norm2[l]) * (1 + sc2) + sh2
        x_next = x_next + gt2 * moe_ffn(h2, *moe_w)
        if l + 1 < DEPTH:
            attn_oc = context_attention(caq, cak, cav, attn_sink[l])
            ctx = ctx + cgt1 * merge_branches(attn_oc, gla_oc, cgr, cga, cgg, *merge_w)
            hc2 = rms_norm(ctx, norm2[l]) * (1 + csc2) + csh2
            ctx = ctx + cgt2 * moe_ffn(hc2, *moe_w)
        x = x_next
    return x
```

```python
import numpy as np
from contextlib import ExitStack
import concourse.bass as bass
import concourse.mybir as mybir
from concourse.bass_utils import run_bass_kernel_spmd

F32 = mybir.dt.float32
BF16 = mybir.dt.bfloat16
U8 = mybir.dt.uint8
I32 = mybir.dt.int32
U32 = mybir.dt.uint32
CAP = 1024
TRASH = 32 * CAP
AF = mybir.ActivationFunctionType
ALU = mybir.AluOpType
AX = mybir.AxisListType

ENGS = ("pe", "act", "dve", "pool", "sp")
DEBUG = False
GLA_LEVEL = 9
GLA_TILES = 99
ACT_SEL = 15
OFFS = {}
N_EXP = 32


class Op:
    __slots__ = ("eng", "fn", "deps", "idx", "signal", "dma", "group", "gcount", "name", "scount")


class Prog:
    def __init__(self, nc, same_engine_sync=True):
        self.nc = nc
        self.ops = {e: [] for e in ENGS}
        self.last_write = {}
        self.readers = {}
        self.group_total = {}
        self.group_waitall = set()
        self.same_engine_sync = same_engine_sync
        self.out_groups = set()

    @staticmethod
    def _norm(keys):
        out = []
        for k in keys:
            if isinstance(k, str) and len(k) >= 2 and k[0] == "b" and k[1].isdigit():
                k = "bank" + k[1]
            out.append(k)
        return tuple(out)

    def _add(self, eng, fn, reads, writes, dma, group, name):
        reads = self._norm(reads); writes = self._norm(writes)
        writes = writes + tuple(k for k in reads if isinstance(k, str) and k.startswith("bank") and k not in writes)
        o = Op()
        o.eng = eng; o.fn = fn; o.deps = set(); o.signal = False
        o.dma = dma; o.group = group; o.gcount = 0; o.name = name; o.scount = None
        for k in reads:
            w = self.last_write.get(k)
            if w is not None:
                o.deps.add(w)
        for k in writes:
            w = self.last_write.get(k)
            if w is not None:
                o.deps.add(w)
            for r in self.readers.get(k, ()):
                o.deps.add(r)
        for k in writes:
            self.last_write[k] = o
            self.readers[k] = []
        for k in reads:
            self.readers.setdefault(k, []).append(o)
        o.deps.discard(o)
        o.idx = len(self.ops[eng])
        self.ops[eng].append(o)
        if dma:
            self.group_total[group] = self.group_total.get(group, 0) + 1
            o.gcount = self.group_total[group]
        return o

    def op(self, eng, fn, reads=(), writes=(), name=None):
        return self._add(eng, fn, tuple(reads), tuple(writes), False, None, name)

    def dma(self, eng, fn, reads=(), writes=(), group=None, waitall=False, is_output=False, name=None):
        assert group is not None
        if waitall:
            self.group_waitall.add(group)
        if is_output:
            self.out_groups.add(group)
        return self._add(eng, fn, tuple(reads), tuple(writes), True, group, name)

    def barrier(self):
        lasts = [self.ops[e][-1] for e in ENGS if self.ops[e]]
        dmas = {}
        for e in ENGS:
            for o in self.ops[e]:
                if o.dma:
                    dmas[o.group] = o
        deps = [d for d in lasts if d.fn is not None] + list(dmas.values())
        for e in ENGS:
            o = Op()
            o.eng = e; o.fn = None; o.deps = set(deps); o.signal = False
            o.dma = False; o.group = None; o.gcount = 0; o.name = "barrier"; o.scount = None
            o.idx = len(self.ops[e])
            self.ops[e].append(o)
        self.last_write = {}
        self.readers = {}

    def _same_skip(self, d, e):
        return d.eng == e and not (self.same_engine_sync and e in ("act", "dve", "pool"))

    def emit(self):
        nc = self.nc
        for e in ENGS:
            for o in self.ops[e]:
                for d in o.deps:
                    if not d.dma and d.fn is not None and not self._same_skip(d, e):
                        d.signal = True
        with ExitStack() as es:
            esem = {e: es.enter_context(nc.semaphore("s_" + e)) for e in ENGS}
            gsem = {g: es.enter_context(nc.semaphore("g_%d" % i))
                    for i, g in enumerate(sorted(self.group_total, key=str))}
            for e in ENGS:
                c = 0
                for o in self.ops[e]:
                    if o.signal and not o.dma:
                        c += 1
                    o.scount = c
            block = es.enter_context(nc.Block())

            def run(e, engobj):
                waited = {}
                for o in self.ops[e]:
                    need = {}
                    for d in o.deps:
                        if d.dma:
                            g = d.group
                            v = 16 * (self.group_total[g] if g in self.group_waitall else d.gcount)
                            key = ("g", g)
                        else:
                            if self._same_skip(d, e):
                                continue
                            if d.fn is None:
                                continue
                            key = ("e", d.eng)
                            v = d.scount
                        need[key] = max(need.get(key, 0), v)
                    for key, v in need.items():
                        if waited.get(key, 0) >= v or v == 0:
                            continue
                        waited[key] = v
                        sem = gsem[key[1]] if key[0] == "g" else esem[key[1]]
                        engobj.wait_ge(sem, v)
                    if o.fn is None:
                        continue
                    ins = o.fn(engobj)
                    if o.dma:
                        ins.then_inc(gsem[o.group], 16)
                    elif o.signal:
                        ins.then_inc(esem[e], 1)
                if e == "sp":
                    for g in sorted(self.out_groups, key=str):
                        engobj.wait_ge(gsem[g], 16 * self.group_total[g])

            block.tensor(lambda t: run("pe", t))
            block.scalar(lambda t: run("act", t))
            block.vector(lambda t: run("dve", t))
            block.gpsimd(lambda t: run("pool", t))
            block.sync(lambda t: run("sp", t))


class Arena:
    def __init__(self, ar, size):
        self.ar = ar; self.size = size; self.top = 0

    def alloc(self, shape, dt):
        nel = int(np.prod(shape))
        esz = 4 if dt in (F32, I32, U32) else 2
        nb = (nel * esz + 63) // 64 * 64
        assert self.top + nb <= self.size, ("arena overflow", self.top, nb, self.size)
        v = self.ar[:, self.top:self.top + nel * esz].bitcast(dt)
        self.top += nb
        if len(shape) == 2:
            v = v.rearrange("p (a b) -> p a b", a=shape[0])
        elif len(shape) == 3:
            v = v.rearrange("p (a b c) -> p a b c", a=shape[0], b=shape[1])
        return v

    def mark(self):
        return self.top

    def release(self, m):
        self.top = m


def build(dbg=False, n_exp_run=N_EXP, stop_after=99):
    nc = bass.Bass("TRN2", target_bir_lowering=False)
    T, LC, D = 2048, 256, 1024
    NT, NCT = 16, 2
    NTT = NT + NCT

    def din(name, shape, dt=F32):
        return nc.dram_tensor(name, list(shape), dt, kind="ExternalInput").ap()

    x_d = din("x", [T, D]); ctx_d = din("ctx", [LC, D]); cvec_d = din("cvec", [128, 16])
    wmod_d = din("w_mod", [D, 6144]); bmodT_d = din("b_modT", [128, 16]); bmodb_d = din("b_mod_b", [128, 4096])
    n1T_d = din("norm1T", [128, 8]); n2b_d = din("norm2_b", [128, D])
    win_d = din("w_in", [D, 4384]); qkg_d = din("qk_gain_b", [128, 128]); sink_d = din("sink_b", [128, 8])
    waf_d = din("w_alpha_f_aug", [32, 256]); wab_d = din("w_alpha_b_aug", [32, 256])
    gnorm_d = din("gla_norm_b", [128, 128])
    wba_d = din("w_branch_attn", [512, D]); wbg_d = din("w_branch_gla", [512, D]); wout_d = din("w_out", [D, D])
    wr_d = din("w_router", [D, 32]); brb_d = din("b_router_b", [128, 32])
    w1_d = din("w_exp_in", [N_EXP, D, 2048]); b1T_d = din("bias1T", [128, N_EXP * 16])
    w2_d = din("w_exp_out", [N_EXP, D, D]); b2_d = din("b_exp_out", [N_EXP, D])
    ropeC_d = din("ropeC", [NT, 128, 640]); ropeS_d = din("ropeS", [NT, 128, 640])
    hm_d = din("hm", [128, 2])
    moec_d = din("moe_c", [128, 64])
    RB = 32 * CAP + 128
    xs_d = nc.dram_tensor("xs_buf", [RB, D], BF16, kind="Internal").ap()
    ys_d = nc.dram_tensor("ys_buf", [RB, D], F32, kind="Internal").ap()
    cmask_d = din("cmask", [128, 6 * 128])
    out_d = nc.dram_tensor("out", [T, D], F32, kind="ExternalOutput").ap()
    x1_d = nc.dram_tensor("x1_buf", [T, D], F32, kind="Internal").ap()
    dbg_d = {}
    if dbg:
        for nm, shp in (("d_hT", [128, 8 * 2304]), ("d_attn_oT", [128, 4 * 2048]), ("d_oglaT", [128, 4 * 2048]),
                        ("d_yT", [128, 8 * 2048]), ("d_W", [128, 16 * 32]), ("d_h2T", [128, 8 * 2048]),
                        ("d_mod", [128, 4 * 1024 + 32]), ("d_QT", [128, 4 * 2048]), ("d_KT", [128, 2304])):
            dbg_d[nm] = nc.dram_tensor(nm, shp, F32, kind="ExternalOutput").ap()

    ARENA = 207 * 1024
    es = ExitStack()
    ar_t = es.enter_context(nc.sbuf_tensor("arena", [128, ARENA], U8))
    psum_t = es.enter_context(nc.psum_tensor("psum", [128, 4096], F32))
    A = Arena(ar_t, ARENA)
    P = Prog(nc)

    def finish():
        P.emit()
        es.close()
        return nc

    def bank(b):
        return psum_t[:, b * 512:(b + 1) * 512]

    def bank_bf(b):
        return psum_t[:, b * 512:(b + 1) * 512].bitcast(BF16)

    def MM(out, lhsT, rhs, start, stop, r, w):
        P.op("pe", lambda e: e.matmul(out, lhsT, rhs, start=start, stop=stop), r, w)

    def TR(out, in_, ident, r, w):
        P.op("pe", lambda e: e.transpose(out, in_, ident), r, w)

    def ACTV(out, in_, func, r, w, bias=None, scale=None, accum=None, eng="act"):
        kw = {}
        if bias is not None:
            kw["bias"] = bias
        if scale is not None:
            kw["scale"] = scale
        if accum is not None:
            kw["accum_out"] = accum
        P.op(eng, lambda e: e.activation(out=out, in_=in_, func=func, **kw), r, w)

    def TT(eng, out, in0, in1, op, r, w):
        P.op(eng, lambda e: e.tensor_tensor(out=out, in0=in0, in1=in1, op=op), r, w)

    def TS(eng, out, in0, s1, op0, r, w, s2=None, op1=None):
        if op1 is None:
            P.op(eng, lambda e: e.tensor_scalar(out=out, in0=in0, scalar1=s1, scalar2=None, op0=op0), r, w)
        else:
            P.op(eng, lambda e: e.tensor_scalar(out=out, in0=in0, scalar1=s1, scalar2=s2, op0=op0, op1=op1), r, w)

    def STT(eng, out, in0, scalar, in1, op0, op1, r, w):
        P.op(eng, lambda e: e.scalar_tensor_tensor(out=out, in0=in0, scalar=scalar, in1=in1, op0=op0, op1=op1), r, w)

    def CP(eng, out, in_, r, w):
        if eng == "act":
            P.op(eng, lambda e: e.copy(out=out, in_=in_), r, w)
        else:
            P.op(eng, lambda e: e.tensor_copy(out=out, in_=in_), r, w)

    def RED(out, in_, r, w):
        P.op("dve", lambda e: e.tensor_reduce(out=out, in_=in_, axis=AX.X, op=ALU.add), r, w)

    def RECIP(out, in_, r, w):
        P.op("dve", lambda e: e.reciprocal(out=out, in_=in_), r, w)

    def MEMSET(eng, ap, val, w):
        P.op(eng, lambda e: e.memset(ap, val), (), w)

    def DMA(eng, out, in_, r, w, group, waitall=False, is_output=False):
        P.dma(eng, lambda e: e.dma_start(out=out, in_=in_), r, w, group=group, waitall=waitall, is_output=is_output)

    def dump(name, src_ap, shape2, keys, dt):
        if not dbg:
            return
        m = A.mark()
        n = shape2
        done = 0
        tmp = A.alloc([256], F32)
        while done < n:
            c = min(256, n - done)
            CP("dve", tmp[:, 0:c], src_ap[:, done:done + c], list(keys) + ["dumptmp"], ["dumptmp"])
            DMA("sp", dbg_d[name][:, done:done + c], tmp[:, 0:c], ["dumptmp"], ["dumpdram" + name], "dbg_" + name, is_output=True)
            done += c
        A.release(m)

    NU = 6
    ring = A.alloc([NU, 8, 512], BF16)
    cm_f = A.alloc([6, 128], F32)
    cm_b = A.alloc([6, 128], BF16)
    gt2_b = A.alloc([1024], F32)
    W_all = A.alloc([NT, 32], F32)
    m4 = A.alloc([2, 4, 128], BF16)
    dest_i = A.alloc([NT, 4], I32)
    wsel = A.alloc([NT, 4], F32)
    moec = A.alloc([64], F32)
    small = A.alloc([512], F32)
    cvec = small[:, 0:16]; silu_c = small[:, 16:32]
    bmodT = small[:, 32:48]; n1T = small[:, 48:56]
    A1 = small[:, 56:72]
    B1 = small[:, 72:88]
    sinkb = small[:, 88:96]; esink = small[:, 96:104]
    brb = small[:, 104:136]
    modT = small[:, 136:168]
    qkg = small[:, 168:296]
    gnorm = small[:, 296:424]
    hm = small[:, 424:426]
    IDENT_F = cm_f[:, 0, :]; LE_F = cm_f[:, 1, :]; GT_F = cm_f[:, 2, :]; GE_F = cm_f[:, 3, :]; LT_F = cm_f[:, 4, :]; ONES_F = cm_f[:, 5, :]
    IDENT_B = cm_b[:, 0, :]; LE_B = cm_b[:, 1, :]; GE_B = cm_b[:, 3, :]; ONES_B = cm_b[:, 5, :]

    ring_use = [0]

    def load_unit(src_ap, shape3=None, ncols=512):
        s = ring_use[0] % NU
        ring_use[0] += 1
        key = "ring%d" % s
        if shape3 is not None:
            dst = ring[:, s, :, :].rearrange("p a b -> p (a b)").rearrange("p (a b) -> p a b", a=shape3[0])
        else:
            dst = ring[:, s, :, 0:ncols]
        DMA("pool", dst, src_ap, [], [key], "g" + key)
        return dst, key

    def wsrc(d_ap, c0, ncols=512):
        return d_ap[:, c0:c0 + ncols].rearrange("(k p) n -> p k n", p=128)

    DMA("sp", cvec, cvec_d, [], ["cvec"], "const", waitall=True)
    DMA("sp", bmodT, bmodT_d, [], ["bmodT"], "const", waitall=True)
    DMA("sp", n1T, n1T_d, [], ["n1T"], "const", waitall=True)
    DMA("sp", sinkb, sink_d, [], ["sinkb"], "const", waitall=True)
    DMA("sp", brb, brb_d, [], ["brb"], "const", waitall=True)
    DMA("sp", qkg, qkg_d, [], ["qkg"], "const", waitall=True)
    DMA("sp", gnorm, gnorm_d, [], ["gnorm"], "const", waitall=True)
    DMA("sp", hm, hm_d, [], ["hm"], "const", waitall=True)
    DMA("sp", moec, moec_d, [], ["moec"], "const", waitall=True)
    DMA("sp", cm_f.rearrange("p a b -> p (a b)"), cmask_d, [], ["cm_f"], "const", waitall=True)
    CP("dve", cm_b.rearrange("p a b -> p (a b)"), cm_f.rearrange("p a b -> p (a b)"), ["cm_f"], ["cm_b"])
    for i4 in range(4):
        CP("dve", m4[:, 0, i4, :], cm_b[:, 1, :], ["cm_b"], ["m4"])
        CP("dve", m4[:, 1, i4, :], cm_b[:, 3, :], ["cm_b"], ["m4"])
    ACTV(silu_c, cvec, AF.Silu, ["cvec"], ["silu_c"])
    ACTV(esink, sinkb, AF.Exp, ["sinkb"], ["esink"])

    m0 = A.mark()
    modvec = A.alloc([3, 1024], F32)
    gt1_b = modvec[:, 0, :]; A2_b = modvec[:, 1, :]; B2_b = modvec[:, 2, :]
    m0b = A.mark()
    wst = A.alloc([2, 8, 512], F32)
    silu_bc = A.alloc([8, 128], F32)
    bias_t = A.alloc([2, 512], F32)
    n2b = A.alloc([1024], F32)
    DMA("sp", n2b, n2b_d, [], ["n2b"], "const", waitall=True)
    sview = silu_c.rearrange("p (k n) -> p k n", n=2)
    for kc in range(8):
        CP("dve", silu_bc[:, kc, :], sview[:, kc, 0:1].to_broadcast([128, 128]), ["silu_c"], ["silu_bc"])
    for u in range(4):
        s = u % 2
        DMA("sp", wst[:, s, :, :], wsrc(wmod_d, u * 512), [], ["wst%d" % s], "gwst%d" % s)
        for j in range(4):
            jj = u * 4 + j
            for kc in range(8):
                MM(bank(2)[:, jj * 2:jj * 2 + 2], wst[:, s, kc, j * 128:(j + 1) * 128], sview[:, kc, :],
                   kc == 0, kc == 7, ["wst%d" % s, "silu_c"], ["b2mod"])
    bmv = bmodT.unsqueeze(2).to_broadcast([128, 16, 2])
    TT("dve", modT.rearrange("p (j n) -> p j n", n=2), bank(2)[:, 0:32].rearrange("p (j n) -> p j n", n=2), bmv, ALU.add,
       ["b2mod", "bmodT"], ["modT"])
    mT = modT.rearrange("p (j n) -> p j n", n=2)
    for n in range(2):
        STT("dve", A1[:, n * 8:(n + 1) * 8], mT[:, 8:16, n], 1.0, n1T, ALU.add, ALU.mult, ["modT", "n1T"], ["A1_%d" % n])
        CP("dve", B1[:, n * 8:(n + 1) * 8], mT[:, 0:8, n], ["modT"], ["B1_%d" % n])
    for u in range(8):
        s = u % 2
        c0 = 2048 + u * 512
        DMA("sp", wst[:, s, :, :], wsrc(wmod_d, c0), [], ["wst%d" % s], "gwst%d" % s)
        DMA("sp", bias_t[:, s, :], bmodb_d[:, u * 512:(u + 1) * 512], [], ["bias%d" % s], "gbias%d" % s)
        pb = bank(s)
        for kc in range(8):
            MM(pb, silu_bc[:, kc, :], wst[:, s, kc, :], kc == 0, kc == 7, ["wst%d" % s, "silu_bc"], ["pb%d" % s])
        vec = u // 2
        half = (u % 2) * 512
        if vec == 0:
            TT("dve", gt1_b[:, half:half + 512], pb, bias_t[:, s, :], ALU.add, ["pb%d" % s, "bias%d" % s], ["gt1_b%d" % half])
        elif vec == 1:
            TT("dve", B2_b[:, half:half + 512], pb, bias_t[:, s, :], ALU.add, ["pb%d" % s, "bias%d" % s], ["B2_b%d" % half])
        elif vec == 2:
            TT("dve", A2_b[:, half:half + 512], pb, bias_t[:, s, :], ALU.add, ["pb%d" % s, "bias%d" % s], ["A2_t%d" % half])
            STT("dve", A2_b[:, half:half + 512], A2_b[:, half:half + 512], 1.0, n2b[:, half:half + 512], ALU.add, ALU.mult,
                ["A2_t%d" % half, "n2b"], ["A2_b%d" % half])
        else:
            TT("dve", gt2_b[:, half:half + 512], pb, bias_t[:, s, :], ALU.add, ["pb%d" % s, "bias%d" % s], ["gt2_b%d" % half])
    if dbg:
        dump("d_mod", modvec.rearrange("p a b -> p (a b)"), 3072, ["gt1_b0", "gt1_b512", "A2_b0", "A2_b512", "B2_b0", "B2_b512"], F32)
    P.barrier()
    A.release(m0b)

    OFFS['hT'] = A.top
    hT = A.alloc([8, LC + T], BF16)
    m1 = A.mark()
    xt = A.alloc([2, 1024], F32)
    xn = A.alloc([2, 1024], BF16)
    junk = A.alloc([1024], BF16)
    st1 = A.alloc([NTT, 4], F32)
    htmp = A.alloc([8, 128], F32)
    for ti in range(NTT):
        s = ti % 2
        src = ctx_d[ti * 128:(ti + 1) * 128, :] if ti < NCT else x_d[(ti - NCT) * 128:(ti - NCT + 1) * 128, :]
        isctx = 1 if ti < NCT else 0
        DMA("sp", xt[:, s, :], src, [], ["xt%d" % s], "gxt%d" % s)
        ACTV(junk, xt[:, s, :], AF.Square, ["xt%d" % s], ["junk", "ssq%d" % s], accum=st1[:, ti, 0:1])
        ACTV(st1[:, ti, 1:2], st1[:, ti, 0:1], AF.Sqrt, ["ssq%d" % s], ["rms%d" % s], scale=1.0 / D, bias=1e-6)
        RECIP(st1[:, ti, 2:3], st1[:, ti, 1:2], ["rms%d" % s], ["rstd%d" % s])
        TS("dve", xn[:, s, :], xt[:, s, :], st1[:, ti, 2:3], ALU.mult, ["xt%d" % s, "rstd%d" % s], ["xn%d" % s])
        pb = bank_bf(s)
        for kc in range(8):
            TR(pb[:, kc * 128:(kc + 1) * 128], xn[:, s, kc * 128:(kc + 1) * 128], IDENT_B, ["xn%d" % s, "cm_b"], ["ptr%d" % s])
        a_bc = A1[:, isctx * 8:(isctx + 1) * 8].unsqueeze(2).to_broadcast([128, 8, 128])
        b_bc = B1[:, isctx * 8:(isctx + 1) * 8].unsqueeze(2).to_broadcast([128, 8, 128])
        hdst = hT[:, :, ti * 128:(ti + 1) * 128]
        TT("dve", htmp, pb.rearrange("p (k t) -> p k t", k=8), a_bc, ALU.mult, ["ptr%d" % s, "A1_%d" % isctx], ["htmp"])
        TT("dve", hdst, htmp, b_bc, ALU.add, ["htmp", "B1_%d" % isctx], ["hT%d" % ti])
    if dbg:
        dump("d_hT", hT.rearrange("p a b -> p (a b)"), 8 * 2304, ["hT%d" % t for t in range(NTT)], BF16)
    P.barrier()
    A.release(m1)
    if stop_after <= 1:
        return finish()

    mA = A.mark()
    o_glaT = A.alloc([4, T], BF16)
    attn_oT = A.alloc([4, T], BF16)

    m2 = A.mark()
    o_f = A.alloc([NT, 512], BF16)
    gr_s = A.alloc([NT, 512], BF16)
    wlr = A.alloc([8, 32], BF16)
    walpha = A.alloc([2, 256], BF16)
    lrT = A.alloc([128], BF16)
    la = A.alloc([256], F32)
    la_hi = A.alloc([256], BF16); la_lo = A.alloc([256], BF16)
    Eq = A.alloc([256], F32); Ek = A.alloc([256], F32); Eh = A.alloc([256], F32)
    qt_ = A.alloc([256], BF16); kt_ = A.alloc([256], BF16); kh_ = A.alloc([256], BF16)
    v_bf = A.alloc([512], BF16)
    qkT = A.alloc([4, 128], BF16)
    AT = A.alloc([4, 128], BF16)
    qpad = A.alloc([4, 128], BF16)
    S32 = A.alloc([2, 128], F32)
    S_bf = A.alloc([2, 128], BF16)
    gam = A.alloc([4], F32)
    osum = A.alloc([512], F32)
    osq = A.alloc([512], F32)
    ost = A.alloc([16], F32)
    on_bf = A.alloc([512], BF16)
    gnorm4 = A.alloc([4, 128], F32)
    DMA("pool", wlr, win_d[:, 2304:2336].rearrange("(k p) n -> p k n", p=128), [], ["wlr"], "const2", waitall=True)
    DMA("pool", walpha[0:32, 0, :], waf_d, [], ["walpha0"], "const2", waitall=True)
    DMA("pool", walpha[0:32, 1, :], wab_d, [], ["walpha1"], "const2", waitall=True)
    MEMSET("dve", lrT[0:32, :], 1.0, ["lrT"])
    for i4 in range(4):
        CP("dve", gnorm4[:, i4, :], gnorm, ["gnorm"], ["gnorm4"])
    u_qk, k_qk = load_unit(wsrc(win_d, 768))
    u_v, k_v = load_unit(wsrc(win_d, 1280))
    u_r, k_r = load_unit(wsrc(win_d, 1792))

    def gla_pass(direction):
        fwd = direction == 0
        MEMSET("dve", S32.rearrange("p a b -> p (a b)"), 0.0, ["S32"])
        MEMSET("dve", S_bf.rearrange("p a b -> p (a b)"), 0.0, ["S_bf"])
        if fwd:
            order = list(range(NTT))
        else:
            order = [1, 0] + [NCT + n for n in range(NT - 1, -1, -1)]
        MP = cm_b[:, 1, :] if fwd else cm_b[:, 3, :]
        MD = cm_b[:, 2, :] if fwd else cm_b[:, 4, :]
        MA = m4[:, 0, :, :] if fwd else m4[:, 1, :, :]
        lc0 = 0 if fwd else 16
        for ti in order[:GLA_TILES]:
            isctx = ti < NCT
            n = ti - NCT
            hcol = slice(ti * 128, (ti + 1) * 128)
            hk = "hT%d" % ti
            for kc in range(8):
                MM(bank(0), hT[:, kc, hcol], u_qk[:, kc, :], kc == 0, kc == 7, [k_qk], ["b0"])
            for kc in range(8):
                MM(bank(1), hT[:, kc, hcol], u_v[:, kc, :], kc == 0, kc == 7, [k_v], ["b1"])
            if fwd and not isctx:
                for kc in range(8):
                    MM(bank(2), hT[:, kc, hcol], u_r[:, kc, :], kc == 0, kc == 7, [k_r], ["b2"])
                ACTV(gr_s[:, n, :], bank(2), AF.Silu, ["b2"], ["gr_s%d" % n])
            for kc in range(8):
                MM(bank(3)[0:16, 0:128], wlr[:, kc, lc0:lc0 + 16], hT[:, kc, hcol], kc == 0, kc == 7, ["wlr"], ["b3a"])
            CP("dve", lrT[0:16, :], bank(3)[0:16, 0:128], ["b3a"], ["lrT"])
            CP("act", v_bf, bank(1), ["b1"], ["v_bf"])
            if GLA_LEVEL <= 0:
                continue
            MM(bank(3)[:, 256:512], lrT[0:32, :], walpha[0:32, direction, :], True, True, ["lrT", "walpha%d" % direction], ["b3z"])
            ACTV(la, bank(3)[:, 256:512], AF.Exp, ["b3z"], ["la_e", "la"], scale=-1.0)
            ACTV(la, la, AF.Ln, ["la_e"], ["la"], bias=1.0)
            if GLA_LEVEL <= 1:
                continue
            CP("act", la_hi, la, ["la"], ["la_hi"])
            TT("dve", la_lo, la, la_hi, ALU.subtract, ["la", "la_hi"], ["la_lo"])
            MM(bank(4)[:, 0:256], MP, la_hi, True, False, ["la_hi", "cm_b"], ["b4p"])
            MM(bank(4)[:, 0:256], MP, la_lo, False, True, ["la_lo", "cm_b"], ["b4p"])
            MM(bank(4)[:, 256:512], MD, la_hi, True, False, ["la_hi", "cm_b"], ["b4d"])
            MM(bank(4)[:, 256:512], MD, la_lo, False, True, ["la_lo", "cm_b"], ["b4d"])
            if GLA_LEVEL <= 1.3:
                continue
            for pr in range(2):
                MM(bank(5)[:, 256 + 2 * pr:258 + 2 * pr], la_hi[:, pr * 128:(pr + 1) * 128], ONES_B[:, 0:2], True, False, ["la_hi", "cm_b"], ["b5g%d" % pr])
                MM(bank(5)[:, 256 + 2 * pr:258 + 2 * pr], la_lo[:, pr * 128:(pr + 1) * 128], ONES_B[:, 0:2], False, True, ["la_lo", "cm_b"], ["b5g%d" % pr])
            if GLA_LEVEL <= 1.6:
                continue
            if ACT_SEL & 1:
                ACTV(Eq, bank(4)[:, 0:256], AF.Exp, ["b4p"], ["Eq"], scale=-1.0 / 16)
            if ACT_SEL & 2:
                ACTV(Ek, bank(4)[:, 0:256], AF.Exp, ["b4p"], ["Ek"], scale=1.0 / 16)
            if ACT_SEL & 4:
                ACTV(Eh, bank(4)[:, 256:512], AF.Exp, ["b4d"], ["Eh"], scale=-1.0 / 16)
            if ACT_SEL & 8:
                ACTV(gam[:, 0:4], bank(5)[:, 256:260], AF.Exp, ["b5g0", "b5g1"], ["gam"], scale=-1.0 / 16)
            if GLA_LEVEL <= 2:
                continue
            if not isctx:
                STT("dve", qt_, bank(0)[:, 0:256], 0.125, Eq, ALU.mult, ALU.mult, ["b0", "Eq"], ["qt_"])
                TT("dve", kt_, bank(0)[:, 256:512], Ek, ALU.mult, ["b0", "Ek"], ["kt_"])
            TT("dve", kh_, bank(0)[:, 256:512], Eh, ALU.mult, ["b0", "Eh"], ["kh_"])
            if not isctx:
                pbt = bank_bf(5)
                for pr in range(2):
                    TR(pbt[:, pr * 128:(pr + 1) * 128], qt_[:, pr * 128:(pr + 1) * 128], IDENT_B, ["qt_", "cm_b"], ["b5t"])
                    TR(pbt[:, (2 + pr) * 128:(3 + pr) * 128], kt_[:, pr * 128:(pr + 1) * 128], IDENT_B, ["kt_", "cm_b"], ["b5t"])
                CP("act", qkT.rearrange("p a b -> p (a b)"), pbt[:, 0:512], ["b5t"], ["qkT"])
                if GLA_LEVEL <= 2.4:
                    continue
                qp4 = qpad.rearrange("p (pr two) t -> p pr two t", two=2)
                for half in range(2):
                    TS("dve", qp4[:, :, half, :], pbt[:, 0:256].rearrange("p (pr t) -> p pr t", pr=2), hm[:, half:half + 1], ALU.mult,
                       ["b5t", "hm"], ["qpad"])
                if GLA_LEVEL <= 2.45:
                    continue
                for h in range(4):
                    MM(bank(6)[:, h * 128:(h + 1) * 128], qkT[:, 2 + h // 2, :], qpad[:, h, :], True, True, ["qkT", "qpad"], ["b6"])
                if GLA_LEVEL <= 2.5:
                    continue
                TT("dve", AT, bank(6).rearrange("p (h i) -> p h i", h=4), MA, ALU.mult, ["b6", "m4"], ["AT"])
                if GLA_LEVEL <= 2.6:
                    continue
                for h in range(4):
                    MM(bank(7)[:, h * 128:(h + 1) * 128], AT[:, h, :], v_bf[:, h * 128:(h + 1) * 128], True, False, ["AT", "v_bf"], ["b7"])
                    MM(bank(7)[:, h * 128:(h + 1) * 128], qpad[:, h, :], S_bf[:, h // 2, :], False, True, ["qpad", "S_bf"], ["b7"])
                if fwd:
                    CP("act", o_f[:, n, :], bank(7), ["b7"], ["o_f%d" % n])
                else:
                    TT("dve", osum, bank(7), o_f[:, n, :], ALU.add, ["b7", "o_f%d" % n], ["osum"])
                    TT("dve", osq, osum, osum, ALU.mult, ["osum"], ["osq"])
                    RED(ost[:, 0:4], osq.rearrange("p (h d) -> p h d", h=4), ["osq"], ["ost0"])
                    ACTV(ost[:, 4:8], ost[:, 0:4], AF.Sqrt, ["ost0"], ["ost1"], scale=1.0 / 128, bias=1e-6)
                    RECIP(ost[:, 8:12], ost[:, 4:8], ["ost1"], ["ost2"])
                    o3 = osum.rearrange("p (h d) -> p h d", h=4)
                    TT("dve", o3, o3, ost[:, 8:12].unsqueeze(2).to_broadcast([128, 4, 128]), ALU.mult, ["osum", "ost2"], ["osum"])
                    TT("dve", o3, o3, gnorm4, ALU.mult, ["osum", "gnorm4"], ["osum"])
                    TT("dve", on_bf, osum, gr_s[:, n, :], ALU.mult, ["osum", "gr_s%d" % n], ["on_bf"])
                    pbo = bank_bf(6)
                    for k4 in range(4):
                        TR(pbo[:, k4 * 128:(k4 + 1) * 128], on_bf[:, k4 * 128:(k4 + 1) * 128], IDENT_B, ["on_bf", "cm_b"], ["b6"])
                    CP("act", o_glaT[:, :, n * 128:(n + 1) * 128], pbo[:, 0:512].rearrange("p (k t) -> p k t", k=4), ["b6"], ["o_glaT%d" % n])
            if GLA_LEVEL <= 3:
                continue
            for pr in range(2):
                MM(bank(1)[:, pr * 256:(pr + 1) * 256], kh_[:, pr * 128:(pr + 1) * 128], v_bf[:, pr * 256:(pr + 1) * 256], True, True,
                   ["kh_", "v_bf"], ["b1"])
            for pr in range(2):
                TS("dve", S32[:, pr, :], S32[:, pr, :], gam[:, 2 * pr:2 * pr + 1], ALU.mult, ["S32", "gam"], ["S32"])
                for half in range(2):
                    c0 = pr * 256 + half * 128
                    STT("dve", S32[:, pr, :], bank(1)[:, c0:c0 + 128], hm[:, half:half + 1], S32[:, pr, :], ALU.mult, ALU.add,
                        ["S32", "hm", "b1"], ["S32"])
            CP("act", S_bf.rearrange("p a b -> p (a b)"), S32.rearrange("p a b -> p (a b)"), ["S32"], ["S_bf"])

    gla_pass(0)
    gla_pass(1)
    if dbg:
        dump("d_oglaT", o_glaT.rearrange("p a b -> p (a b)"), 4 * 2048, ["o_glaT%d" % t for t in range(NT)], BF16)
    P.barrier()
    A.release(m2)
    if stop_after <= 2:
        return finish()

    m3 = A.mark()
    QT = A.alloc([4, T], BF16)
    KT = A.alloc([2, LC + T], BF16)
    Vaug = A.alloc([NTT, 2, 66], BF16)
    ropeC = A.alloc([2, 640], F32); ropeS = A.alloc([2, 640], F32)
    qg10 = A.alloc([10, 64], F32)
    qst = A.alloc([40], F32)
    qn = A.alloc([640], F32)
    t1 = A.alloc([640], F32)
    t2 = A.alloc([640], F32)
    sq = t2
    qr_bf = A.alloc([640], BF16)
    PT = A.alloc([2, 5, 512], BF16)
    ao = A.alloc([512], BF16)
    den = A.alloc([8], F32)
    for i10 in range(10):
        CP("dve", qg10[:, i10, :], qkg[:, 0:64] if i10 < 8 else qkg[:, 64:128], ["qkg"], ["qg10"])
    MEMSET("dve", Vaug.rearrange("p a b c -> p (a b c)"), 1.0, ["Vaug"])
    u_q, k_q = load_unit(wsrc(win_d, 0))
    u_kv, k_kv = load_unit(wsrc(win_d, 512, 256), ncols=256)
    for ti in range(NTT):
        isctx = ti < NCT
        n = ti - NCT
        hcol = slice(ti * 128, (ti + 1) * 128)
        for kc in range(8):
            MM(bank(1)[:, 0:256], hT[:, kc, hcol], u_kv[:, kc, 0:256], kc == 0, kc == 7, [k_kv], ["b1"])
        if not isctx:
            for kc in range(8):
                MM(bank(0), hT[:, kc, hcol], u_q[:, kc, :], kc == 0, kc == 7, [k_q], ["b0"])
        CP("act", Vaug[:, ti, :, 0:64], bank(1)[:, 128:256].rearrange("p (k d) -> p k d", k=2), ["b1", "Vaug"], ["Vaug%d" % ti])
        nh = 2 if isctx else 10
        c_lo = 512 if isctx else 0
        if not isctx:
            ACTV(sq[:, 0:512], bank(0), AF.Square, ["b0"], ["sq", "t2a", "t2b"])
        ACTV(sq[:, 512:640], bank(1)[:, 0:128], AF.Square, ["b1"], ["sq", "t2a", "t2b"])
        hs = slice(c_lo // 64, 10)
        RED(qst[:, hs], sq[:, c_lo:640].rearrange("p (h d) -> p h d", d=64), ["sq"], ["qst0"])
        ACTV(qst[:, 10 + c_lo // 64:20], qst[:, hs], AF.Sqrt, ["qst0"], ["qst1"], scale=1.0 / 64, bias=1e-6)
        RECIP(qst[:, 20 + c_lo // 64:30], qst[:, 10 + c_lo // 64:20], ["qst1"], ["qst2"])
        if not isctx:
            TT("dve", qn[:, 0:512].rearrange("p (h d) -> p h d", d=64), bank(0).rearrange("p (h d) -> p h d", d=64),
               qst[:, 20:28].unsqueeze(2).to_broadcast([128, 8, 64]), ALU.mult, ["b0", "qst2"], ["qn_q"])
            TT("dve", qn[:, 0:512].rearrange("p (h d) -> p h d", d=64), qn[:, 0:512].rearrange("p (h d) -> p h d", d=64),
               qg10[:, 0:8, :], ALU.mult, ["qn_q", "qg10"], ["qn_q"])
        TT("dve", qn[:, 512:640].rearrange("p (h d) -> p h d", d=64), bank(1)[:, 0:128].rearrange("p (h d) -> p h d", d=64),
           qst[:, 28:30].unsqueeze(2).to_broadcast([128, 2, 64]), ALU.mult, ["b1", "qst2"], ["qn_k"])
        if isctx:
            TT("dve", qr_bf[:, 512:640].rearrange("p (h d) -> p h d", d=64), qn[:, 512:640].rearrange("p (h d) -> p h d", d=64),
               qg10[:, 8:10, :], ALU.mult, ["qn_k", "qg10"], ["qr_k"])
        else:
            TT("dve", qn[:, 512:640].rearrange("p (h d) -> p h d", d=64), qn[:, 512:640].rearrange("p (h d) -> p h d", d=64),
               qg10[:, 8:10, :], ALU.mult, ["qn_k", "qg10"], ["qn_k"])
            rs = n % 2
            DMA("sp", ropeC[:, rs, :], ropeC_d[n], [], ["ropeC%d" % rs], "gropeC%d" % rs)
            DMA("sp", ropeS[:, rs, :], ropeS_d[n], [], ["ropeS%d" % rs], "gropeS%d" % rs)
            TT("dve", t1, qn, ropeC[:, rs, :], ALU.mult, ["qn_q", "qn_k", "ropeC%d" % rs], ["t1"])
            q5 = qn.rearrange("p (h g two s) -> p h g two s", g=2, two=2, s=16)
            t5 = t2.rearrange("p (h g two s) -> p h g two s", g=2, two=2, s=16)
            S5 = ropeS[:, rs, :].rearrange("p (h g two s) -> p h g two s", g=2, two=2, s=16)
            TT("dve", t5[:, :, :, 0, :], q5[:, :, :, 1, :], S5[:, :, :, 0, :], ALU.mult,
               ["qn_q", "qn_k", "ropeS%d" % rs, "sq"], ["t2a", "sq"])
            TT("dve", t5[:, :, :, 1, :], q5[:, :, :, 0, :], S5[:, :, :, 1, :], ALU.mult,
               ["qn_q", "qn_k", "ropeS%d" % rs, "sq"], ["t2b", "sq"])
            TT("dve", qr_bf[:, 0:512].rearrange("p (g kv d) -> p kv g d", g=4, kv=2), t1[:, 0:512].rearrange("p (kv g d) -> p kv g d", kv=2, g=4),
               t2[:, 0:512].rearrange("p (kv g d) -> p kv g d", kv=2, g=4), ALU.add, ["t1", "t2a", "t2b"], ["qr_q"])
            TT("dve", qr_bf[:, 512:640], t1[:, 512:640], t2[:, 512:640], ALU.add, ["t1", "t2a", "t2b"], ["qr_k"])
        pbt = bank_bf(2 + ti % 2)
        pk = "b%dt" % (2 + ti % 2)
        TR(pbt[:, 512:640], qr_bf[:, 512:640], IDENT_B, ["qr_k", "cm_b"], [pk])
        if not isctx:
            for j in range(4):
                TR(pbt[:, j * 128:(j + 1) * 128], qr_bf[:, j * 128:(j + 1) * 128], IDENT_B, ["qr_q", "cm_b"], [pk])
            CP("act", QT[:, :, n * 128:(n + 1) * 128], pbt[:, 0:512].rearrange("p (j t) -> p j t", j=4), [pk], ["QT%d" % n])
        TS("dve", KT[:, 0, hcol], pbt[:, 512:640], hm[:, 0:1], ALU.mult, [pk, "hm"], ["KT%d" % ti])
        TS("dve", KT[:, 1, hcol], pbt[:, 512:640], hm[:, 1:2], ALU.mult, [pk, "hm", "KT%d" % ti], ["KT%d" % ti])
    if dbg:
        dump("d_QT", QT.rearrange("p a b -> p (a b)"), 4 * 2048, ["QT%d" % t for t in range(NT)], BF16)
        dump("d_KT", KT[:, 0, :], 2304, ["KT%d" % t for t in range(NTT)], BF16)
    it = 0
    for n in range(NT):
        keyt = [(0, None), (1, None)]
        if n > 0:
            keyt.append((NCT + n - 1, m4[:, 1, :, :]))
        keyt.append((NCT + n, None))
        if n < NT - 1:
            keyt.append((NCT + n + 1, m4[:, 0, :, :]))
        for kv in range(2):
            ps_ = slice(kv * 64, kv * 64 + 64)
            sl = it % 2
            it += 1
            for si, (kt, msk) in enumerate(keyt):
                b = (4, 5, 6, 1, 2)[si % 5]
                MM(bank(b), KT[:, kv, kt * 128:(kt + 1) * 128], QT[:, :, n * 128:(n + 1) * 128], True, True,
                   ["KT%d" % kt, "QT%d" % n], ["b%d" % b])
                ACTV(PT[:, sl, si, :], bank(b), AF.Exp, ["b%d" % b], ["PT%d_%d" % (sl, si)], scale=0.125)
                if msk is not None:
                    p3 = PT[:, sl, si, :].rearrange("p (g q) -> p g q", g=4)
                    TT("dve", p3, p3, msk, ALU.mult, ["PT%d_%d" % (sl, si), "m4"], ["PT%d_%d" % (sl, si)])
            pvb = (7, 0)[it % 2]
            pob = bank(pvb)
            for g in range(4):
                for si, (kt, msk) in enumerate(keyt):
                    MM(pob[:, g * 66:g * 66 + 65], PT[:, sl, si, g * 128:(g + 1) * 128], Vaug[:, kt, kv, 0:65],
                       si == 0, si == len(keyt) - 1, ["PT%d_%d" % (sl, si), "Vaug%d" % kt], ["b%d" % pvb])
            po3 = pob[:, 0:264].rearrange("p (g c) -> p g c", g=4)
            TT("dve", den[:, 0:4], po3[:, :, 64], esink[:, kv * 4:(kv + 1) * 4], ALU.add, ["b%d" % pvb, "esink"], ["den0"])
            RECIP(den[:, 4:8], den[:, 0:4], ["den0"], ["den1"])
            TT("dve", ao[:, kv * 256:(kv + 1) * 256].rearrange("p (g d) -> p g d", g=4), po3[:, :, 0:64],
               den[:, 4:8].unsqueeze(2).to_broadcast([128, 4, 64]), ALU.mult, ["b%d" % pvb, "den1"], ["ao%d" % kv])
        pbo = bank_bf(3)
        for k4 in range(4):
            TR(pbo[:, k4 * 128:(k4 + 1) * 128], ao[:, k4 * 128:(k4 + 1) * 128], IDENT_B, ["ao0", "ao1", "cm_b"], ["b3t"])
        CP("act", attn_oT[:, :, n * 128:(n + 1) * 128], pbo[:, 0:512].rearrange("p (k t) -> p k t", k=4), ["b3t"], ["attn_oT%d" % n])
    if dbg:
        dump("d_attn_oT", attn_oT.rearrange("p a b -> p (a b)"), 4 * 2048, ["attn_oT%d" % t for t in range(NT)], BF16)
    P.barrier()
    A.release(m3)
    if stop_after <= 3:
        return finish()

    yT_start = A.mark()
    yT = A.alloc([8, T], BF16)
    m4 = A.mark()
    sga = A.alloc([2, 512], F32); sgg = A.alloc([2, 512], F32)
    ty1 = A.alloc([2, 512], F32); ty2 = A.alloc([2, 512], F32)
    u_ba, k_ba = load_unit(wba_d.rearrange("(k p) n -> p k n", p=128), shape3=[4, 1024])
    u_bg, k_bg = load_unit(wbg_d.rearrange("(k p) n -> p k n", p=128), shape3=[4, 1024])
    cnt = 0
    for dc in range(8):
        if dc % 4 == 0:
            u_ga, k_ga = load_unit(wsrc(win_d, 2336 + (dc // 4) * 512))
            u_gg, k_gg = load_unit(wsrc(win_d, 3360 + (dc // 4) * 512))
        mcol = slice((dc % 4) * 128, (dc % 4) * 128 + 128)
        for tb in range(4):
            s = cnt % 2
            cnt += 1
            tcol = slice(LC + tb * 512, LC + (tb + 1) * 512)
            ocol = slice(tb * 512, (tb + 1) * 512)
            hk = ["hT%d" % t for t in range(NCT + tb * 4, NCT + tb * 4 + 4)]
            b_ga, b_gg, b_p1, b_p2 = (0, 1, 2, 3) if s == 0 else (4, 5, 6, 7)
            for kc in range(8):
                MM(bank(b_ga), u_ga[:, kc, mcol], hT[:, kc, tcol], kc == 0, kc == 7, [k_ga], ["b%d" % b_ga])
            for kc in range(8):
                MM(bank(b_gg), u_gg[:, kc, mcol], hT[:, kc, tcol], kc == 0, kc == 7, [k_gg], ["b%d" % b_gg])
            for k4 in range(4):
                MM(bank(b_p1), u_ba[:, k4, dc * 128:(dc + 1) * 128], attn_oT[:, k4, ocol], k4 == 0, k4 == 3, [k_ba], ["b%d" % b_p1])
            for k4 in range(4):
                MM(bank(b_p2), u_bg[:, k4, dc * 128:(dc + 1) * 128], o_glaT[:, k4, ocol], k4 == 0, k4 == 3, [k_bg], ["b%d" % b_p2])
            ACTV(sga[:, s, :], bank(b_ga), AF.Sigmoid, ["b%d" % b_ga], ["sga%d" % s])
            ACTV(sgg[:, s, :], bank(b_gg), AF.Sigmoid, ["b%d" % b_gg], ["sgg%d" % s])
            TT("dve", ty1[:, s, :], bank(b_p1), sga[:, s, :], ALU.mult, ["b%d" % b_p1, "sga%d" % s], ["ty1%d" % s])
            TT("dve", ty2[:, s, :], bank(b_p2), sgg[:, s, :], ALU.mult, ["b%d" % b_p2, "sgg%d" % s], ["ty2%d" % s])
            TT("pool", yT[:, dc, ocol], ty1[:, s, :], ty2[:, s, :], ALU.add, ["ty1%d" % s, "ty2%d" % s], ["yT"])
    if dbg:
        dump("d_yT", yT.rearrange("p a b -> p (a b)"), 8 * 2048, ["yT"], BF16)
    P.barrier()
    A.release(m4)
    if stop_after <= 4:
        return finish()

    h2T = hT[:, :, 0:T]
    m5 = A.mark()
    A.release(mA)
    xt2 = A.alloc([2, 1024], F32)
    x1 = A.alloc([2, 1024], F32)
    h2 = A.alloc([1, 1024], F32)
    h2hi = A.alloc([1024], BF16); h2lo = A.alloc([1024], BF16)
    h2Tlo = A.alloc([8, 128], BF16)
    wr_f = A.alloc([8, 32], F32)
    wr_hi = A.alloc([8, 32], BF16); wr_lo = A.alloc([8, 32], BF16)
    junk2 = A.alloc([1024], BF16)
    st2 = A.alloc([NT, 4], F32)
    lg = A.alloc([32], F32); top8 = A.alloc([8], F32); rt = A.alloc([8], F32)
    msk_t = A.alloc([32], F32); ex_t = A.alloc([32], F32)
    cum = A.alloc([32], F32); rk = A.alloc([32], F32); okm = A.alloc([32], F32); dstf = A.alloc([32], F32)
    m_bf = A.alloc([32], BF16); oh = A.alloc([32], F32)
    w8 = A.alloc([8], F32); i8 = A.alloc([8], U32); i8f = A.alloc([8], F32); dsel = A.alloc([4], F32)
    MEMSET("dve", cum, 0.0, ["cum"])
    DMA("sp", wr_f, wr_d.rearrange("(k p) n -> p k n", p=128), [], ["wr_f"], "const4", waitall=True)
    CP("act", wr_hi, wr_f, ["wr_f"], ["wr_hi"])
    TT("dve", wr_lo, wr_f, wr_hi, ALU.subtract, ["wr_f", "wr_hi"], ["wr_lo"])
    u_o0, k_o0 = load_unit(wsrc(wout_d, 0))
    u_o1, k_o1 = load_unit(wsrc(wout_d, 512))
    for n in range(NT):
        s = n % 2
        DMA("sp", xt2[:, s, :], x_d[n * 128:(n + 1) * 128, :], [], ["xt2%d" % s], "gxt2%d" % s)
        for hf, (uo, ko) in enumerate(((u_o0, k_o0), (u_o1, k_o1))):
            for kc in range(8):
                MM(bank(hf), yT[:, kc, n * 128:(n + 1) * 128], uo[:, kc, :], kc == 0, kc == 7, [ko, "yT"], ["b%d" % hf])
            hc = slice(hf * 512, (hf + 1) * 512)
            TT("dve", x1[:, s, hc], bank(hf), gt1_b[:, hc], ALU.mult, ["b%d" % hf], ["x1t%d_%d" % (s, hf), "x1_%d_%d" % (s, hf)])
            TT("dve", x1[:, s, hc], x1[:, s, hc], xt2[:, s, hc], ALU.add, ["x1t%d_%d" % (s, hf), "xt2%d" % s], ["x1_%d_%d" % (s, hf)])
        x1k = ["x1_%d_0" % s, "x1_%d_1" % s]
        DMA("sp", x1_d[n * 128:(n + 1) * 128, :], x1[:, s, :], x1k, ["x1d%d" % n], "gx1d%d" % s)
        ACTV(junk2, x1[:, s, :], AF.Square, x1k, ["junk2", "s2q%d" % s], accum=st2[:, n, 0:1])
        ACTV(st2[:, n, 1:2], st2[:, n, 0:1], AF.Sqrt, ["s2q%d" % s], ["s2r%d" % s], scale=1.0 / D, bias=1e-6)
        RECIP(st2[:, n, 2:3], st2[:, n, 1:2], ["s2r%d" % s], ["s2s%d" % s])
        STT("dve", h2[:, 0, :], x1[:, s, :], st2[:, n, 2:3], A2_b, ALU.mult, ALU.mult, x1k + ["s2s%d" % s], ["h2t"])
        TT("pool", h2[:, 0, :], h2[:, 0, :], B2_b, ALU.add, ["h2t"], ["h2"])
        CP("act", h2hi, h2[:, 0, :], ["h2"], ["h2hi"])
        TT("dve", h2lo, h2[:, 0, :], h2hi, ALU.subtract, ["h2", "h2hi"], ["h2lo"])
        for kc in range(8):
            TR(bank_bf(2)[:, kc * 128:(kc + 1) * 128], h2hi[:, kc * 128:(kc + 1) * 128], IDENT_B, ["h2hi", "cm_b"], ["b2"])
        for kc in range(8):
            TR(bank_bf(3)[:, kc * 128:(kc + 1) * 128], h2lo[:, kc * 128:(kc + 1) * 128], IDENT_B, ["h2lo", "cm_b"], ["b3"])
        CP("act", h2T[:, :, n * 128:(n + 1) * 128], bank_bf(2).rearrange("p (k t) -> p k t", k=8), ["b2"], ["h2T%d" % n])
        CP("dve", h2Tlo, bank_bf(3).rearrange("p (k t) -> p k t", k=8), ["b3"], ["h2Tlo"])
        for kc in range(8):
            MM(bank(4)[:, 0:32], h2T[:, kc, n * 128:(n + 1) * 128], wr_hi[:, kc, :], kc == 0, False, ["h2T%d" % n, "wr_hi"], ["b4"])
        for kc in range(8):
            MM(bank(4)[:, 0:32], h2Tlo[:, kc, :], wr_hi[:, kc, :], False, False, ["h2Tlo", "wr_hi"], ["b4"])
        for kc in range(8):
            MM(bank(4)[:, 0:32], h2T[:, kc, n * 128:(n + 1) * 128], wr_lo[:, kc, :], False, kc == 7, ["h2T%d" % n, "wr_lo"], ["b4"])
        TT("dve", lg, bank(4)[:, 0:32], brb, ALU.add, ["b4", "brb"], ["lg"])
        P.op("dve", lambda e: e.max(out=top8, in_=lg), ["lg"], ["top8"])
        TS("dve", msk_t, lg, top8[:, 3:4], ALU.is_ge, ["lg", "top8"], ["msk_t"])
        TS("dve", rt[:, 0:1], top8[:, 0:1], -1.0, ALU.mult, ["top8"], ["rt0"])
        ACTV(ex_t, lg, AF.Exp, ["lg", "rt0"], ["ex_t"], bias=rt[:, 0:1], scale=1.0)
        TT("dve", ex_t, ex_t, msk_t, ALU.mult, ["ex_t", "msk_t"], ["ex_t"])
        RED(rt[:, 1:2], ex_t, ["ex_t"], ["rt1"])
        RECIP(rt[:, 2:3], rt[:, 1:2], ["rt1"], ["rt2"])
        TS("dve", W_all[:, n, :], ex_t, rt[:, 2:3], ALU.mult, ["ex_t", "rt2"], ["W_all%d" % n])
        CP("dve", m_bf, msk_t, ["msk_t"], ["m_bf"])
        MM(bank(5)[:, 0:32], cm_b[:, 4, :], m_bf, True, True, ["m_bf", "cm_b"], ["b5"])
        MM(bank(5)[:, 32:64], ONES_B, m_bf, True, True, ["m_bf", "cm_b"], ["b5"])
        TT("dve", rk, bank(5)[:, 0:32], cum, ALU.add, ["b5", "cum"], ["rk"])
        TT("dve", cum, cum, bank(5)[:, 32:64], ALU.add, ["b5", "cum", "rk"], ["cum"])
        TS("dve", okm, rk, float(CAP), ALU.is_lt, ["rk"], ["okm"])
        TT("dve", okm, okm, msk_t, ALU.mult, ["okm", "msk_t"], ["okm"])
        TT("dve", W_all[:, n, :], W_all[:, n, :], okm, ALU.mult, ["W_all%d" % n, "okm"], ["W_all%d" % n])
        TT("dve", dstf, rk, moec[:, 32:64], ALU.add, ["rk", "moec"], ["dstf"])
        TS("dve", dstf, dstf, -float(TRASH), ALU.add, ["dstf"], ["dstf"])
        TT("dve", dstf, dstf, okm, ALU.mult, ["dstf", "okm"], ["dstf"])
        TS("dve", dstf, dstf, float(TRASH), ALU.add, ["dstf"], ["dstf"])
        P.op("dve", lambda e, n=n: e.max(out=w8, in_=W_all[:, n, :]), ["W_all%d" % n], ["w8"])
        P.op("dve", lambda e, n=n: e.max_index(out=i8, in_max=w8, in_values=W_all[:, n, :]), ["W_all%d" % n, "w8"], ["i8"])
        CP("dve", i8f, i8, ["i8"], ["i8f"])
        CP("dve", wsel[:, n, :], w8[:, 0:4], ["w8"], ["wsel%d" % n])
        for k in range(4):
            TS("dve", oh, moec[:, 0:32], i8f[:, k:k + 1], ALU.is_equal, ["moec", "i8f"], ["oh"])
            TT("dve", oh, oh, dstf, ALU.mult, ["oh", "dstf"], ["oh"])
            RED(dsel[:, k:k + 1], oh, ["oh"], ["dsel%d" % k])
        CP("dve", dest_i[:, n, :], dsel, ["dsel%d" % k for k in range(4)], ["dest%d" % n])
        for k in range(4):
            P.dma("pool", lambda e, n=n, k=k: e.indirect_dma_start(
                out=xs_d, out_offset=bass.IndirectOffsetOnAxis(ap=dest_i[:, n, k:k + 1], axis=0),
                in_=h2hi, in_offset=None),
                ["dest%d" % n, "h2hi"], ["xsb"], group="gsc%d" % k)
    assert A.top <= yT_start, (A.top, yT_start)
    if dbg:
        dump("d_W", W_all.rearrange("p a b -> p (a b)"), 512, ["W_all%d" % t for t in range(NT)], F32)
    P.barrier()
    if stop_after <= 5:
        return finish()
    A.release(m0)

    gsu = A.alloc([3, 3, 512], F32)
    gbuf = gsu[:, 0, :, :]; sbuf_ = gsu[:, 1, :, :]; ubuf = gsu[:, 2, :, :]
    NRT = CAP // 128
    xr = A.alloc([2, NRT, 1024], BF16)
    xsT2 = A.alloc([2, 8, CAP], BF16)
    b1T = A.alloc([N_EXP, 16], F32)
    b2p = A.alloc([1024], BF16)
    Wpad = A.alloc([128], BF16)
    WT = A.alloc([NT, 128], BF16)
    gs = A.alloc([8, CAP], BF16)
    actT = A.alloc([8, CAP], BF16)
    yst = A.alloc([6, 512], F32)
    accb = A.alloc([1024], F32)
    fin = gsu.rearrange("p a b c -> p (a b c)")[:, 0:2048].rearrange("p (a b) -> p a b", a=2)
    DMA("sp", b1T.rearrange("p a b -> p (a b)"), b1T_d, [], ["bias1T0"], "const5", waitall=True)
    TS("dve", b1T[:, :, 8:16], b1T[:, :, 8:16], 1.0, ALU.add, ["bias1T0"], ["bias1T"])
    MEMSET("dve", b2p, 0.0, ["b2p"])
    MEMSET("dve", Wpad, 0.0, ["Wpad"])
    MEMSET("dve", accb, 0.0, ["accb"])
    DMA("sp", ys_d[TRASH:TRASH + 128, :], accb, ["accb"], ["ysb"], "gysz")
    DMA("pool", b2p[0:32, :], b2_d, ["b2p"], ["b2p"], "const5b", waitall=True)
    for n in range(NT):
        CP("dve", Wpad[:, 0:32], W_all[:, n, :], ["Wpad"], ["Wpad"])
        TR(bank_bf(0)[:, 0:128], Wpad, IDENT_B, ["Wpad"], ["b0"])
        CP("act", WT[:, n, :], bank_bf(0)[:, 0:128], ["b0"], ["WT%d" % n])
    pcnt = 0
    ycnt = 0
    ecnt = 0
    NCB = CAP // 512

    def load_x(e):
        xs_ = e % 2
        DMA("sp", xr[:, xs_, :, :], xs_d[e * CAP:(e + 1) * CAP, :].rearrange("(r p) d -> p r d", p=128), ["xsb"], ["xr%d" % xs_], "gxr%d" % xs_)

    def load_w(e, u):
        if u < 4:
            return load_unit(w1_d[e, :, u * 512:(u + 1) * 512].rearrange("(k p) n -> p k n", p=128))
        return load_unit(w2_d[e, :, (u - 4) * 512:(u - 3) * 512].rearrange("(k p) n -> p k n", p=128))

    units = {}
    if n_exp_run > 0:
        load_x(0)
        for u in range(6):
            units[(0, u)] = load_w(0, u)
    def tr_task(e, r):
        nonlocal ecnt
        xs_ = e % 2
        tb_ = 4 + (ecnt % 2)
        ecnt += 1
        for kc in range(8):
            TR(bank_bf(tb_)[:, kc * 128:(kc + 1) * 128], xr[:, xs_, r, kc * 128:(kc + 1) * 128], IDENT_B, ["xr%d" % xs_, "cm_b"], ["b%d" % tb_])
        CP("act" if r % 2 == 0 else "dve", xsT2[:, xs_, :, r * 128:(r + 1) * 128], bank_bf(tb_).rearrange("p (k t) -> p k t", k=8),
           ["b%d" % tb_], ["xsT%d_%d" % (xs_, r)])

    if n_exp_run > 0:
        for r in range(NRT):
            tr_task(0, r)
    for e in range(n_exp_run):
        xs_ = e % 2
        xsT = xsT2[:, xs_, :, :]
        if e + 1 < n_exp_run:
            load_x(e + 1)
        for u in range(2):
            uu, uk = units[(e, u)]
            for cb in range(NCB):
                tcol = slice(cb * 512, (cb + 1) * 512)
                hk = ["xsT%d_%d" % (xs_, r) for r in range(cb * 4, cb * 4 + 4)]
                for mm_ in range(4):
                    m = u * 4 + mm_
                    pbk = (0, 1, 2, 3, 6, 7)[pcnt % 6]
                    pcnt += 1
                    s = pcnt % 3
                    for kc in range(8):
                        MM(bank(pbk), uu[:, kc, mm_ * 128:(mm_ + 1) * 128], xsT[:, kc, tcol], kc == 0, kc == 7, [uk] + hk, ["b%d" % pbk])
                    TS("dve", gbuf[:, s, :], bank(pbk), b1T[:, e, m:m + 1], ALU.add, ["b%d" % pbk, "bias1T"], ["gbuf%d" % s], s2=7.0, op1=ALU.min)
                    ACTV(sbuf_[:, s, :], gbuf[:, s, :], AF.Sigmoid, ["gbuf%d" % s], ["sbuf%d" % s], scale=1.702)
                    TT("dve", gs[:, m, tcol], gbuf[:, s, :], sbuf_[:, s, :], ALU.mult, ["gbuf%d" % s, "sbuf%d" % s], ["gs%d_%d" % (m, cb)])
            if e + 1 < n_exp_run:
                units[(e + 1, u)] = load_w(e + 1, u)
        for u in range(2, 4):
            uu, uk = units[(e, u)]
            for cb in range(NCB):
                tcol = slice(cb * 512, (cb + 1) * 512)
                hk = ["xsT%d_%d" % (xs_, r) for r in range(cb * 4, cb * 4 + 4)]
                for mm_ in range(4):
                    m = (u - 2) * 4 + mm_
                    pbk = (0, 1, 2, 3, 6, 7)[pcnt % 6]
                    pcnt += 1
                    s = pcnt % 3
                    for kc in range(8):
                        MM(bank(pbk), uu[:, kc, mm_ * 128:(mm_ + 1) * 128], xsT[:, kc, tcol], kc == 0, kc == 7, [uk] + hk, ["b%d" % pbk])
                    ACTV(ubuf[:, s, :], bank(pbk), AF.Identity, ["b%d" % pbk, "bias1T"], ["ubuf%d" % s], bias=b1T[:, e, 8 + m:9 + m], scale=1.0)
                    TS("dve", ubuf[:, s, :], ubuf[:, s, :], 8.0, ALU.min, ["ubuf%d" % s], ["ubuf%d" % s], s2=-6.0, op1=ALU.max)
                    TT("dve", actT[:, m, tcol], ubuf[:, s, :], gs[:, m, tcol], ALU.mult, ["ubuf%d" % s, "gs%d_%d" % (m, cb)], ["actT%d_%d" % (m, cb)])
            if e + 1 < n_exp_run:
                units[(e + 1, u)] = load_w(e + 1, u)
        for hf in range(2):
            uu, uk = units[(e, 4 + hf)]
            hc = slice(hf * 512, (hf + 1) * 512)
            for r in range(NRT):
                cb = r // 4
                ak = ["actT%d_%d" % (m, cb) for m in range(8)]
                pbk = (6, 7, 0, 1, 2, 3)[ycnt % 6]
                ys_ = ycnt % 6
                ycnt += 1
                for kc in range(8):
                    MM(bank(pbk), actT[:, kc, r * 128:(r + 1) * 128], uu[:, kc, :], kc == 0, kc == 7, [uk] + ak, ["b%d" % pbk])
                CP("act" if ys_ % 2 == 0 else "dve", yst[:, ys_, 0:512], bank(pbk), ["b%d" % pbk], ["yst%d" % ys_])
                r0 = e * CAP + r * 128
                DMA("sp", ys_d[r0:r0 + 128, hc], yst[:, ys_, 0:512], ["yst%d" % ys_], ["ysb"], "gys%d" % ys_)
                if e + 1 < n_exp_run and hf == 0:
                    tr_task(e + 1, r)
            if e + 1 < n_exp_run:
                units[(e + 1, 4 + hf)] = load_w(e + 1, 4 + hf)
    P.barrier()
    xrf = xr.rearrange("p a b c -> p (a b c)")
    gats = [xrf[:, 0:8192].bitcast(F32).rearrange("p (a b) -> p a b", a=4),
            xrf[:, 8192:16384].bitcast(F32).rearrange("p (a b) -> p a b", a=4)]
    for n in range(NT):
        s = n % 2
        gt_ = gats[s]
        DMA("sp", fin[:, s, :], x1_d[n * 128:(n + 1) * 128, :], ["x1d%d" % n], ["fin%d" % s], "gfin%d" % s)
        for k in range(4):
            P.dma("pool", lambda e, n=n, k=k, gt_=gt_: e.indirect_dma_start(
                out=gt_[:, k, :], out_offset=None, in_=ys_d,
                in_offset=bass.IndirectOffsetOnAxis(ap=dest_i[:, n, k:k + 1], axis=0)),
                ["ysb"], ["gat%d_%d" % (s, k)], group="ggat%d_%d" % (s, k))
        TS("dve", accb, gt_[:, 0, :], wsel[:, n, 0:1], ALU.mult, ["gat%d_0" % s], ["accb"])
        for k in range(1, 4):
            STT("dve", accb, gt_[:, k, :], wsel[:, n, k:k + 1], accb, ALU.mult, ALU.add, ["gat%d_%d" % (s, k), "accb"], ["accb"])
        for hf in range(2):
            hc = slice(hf * 512, (hf + 1) * 512)
            MM(bank(hf), WT[:, n, :], b2p[:, hc], True, True, ["WT%d" % n, "b2p"], ["b%d" % hf])
            TT("dve", accb[:, hc], accb[:, hc], bank(hf), ALU.add, ["accb", "b%d" % hf], ["accb"])
        TT("dve", accb, accb, gt2_b, ALU.mult, ["accb"], ["accb"])
        TT("dve", fin[:, s, :], fin[:, s, :], accb, ALU.add, ["fin%d" % s, "accb"], ["fin%d" % s])
        DMA("sp", out_d[n * 128:(n + 1) * 128, :], fin[:, s, :], ["fin%d" % s], ["outd%d" % n], "gout%d" % s, is_output=True)
    P.emit()
    es.close()
    return nc


def _host_consts():
    T = 2048
    t = np.arange(T)
    inv = 10000.0 ** (-np.arange(0, 32, 2, dtype=np.float64) / 32)
    ar = (t // 64)[:, None] * inv
    ac = (t % 64)[:, None] * inv
    C = np.concatenate([np.cos(ar), np.cos(ar), np.cos(ac), np.cos(ac)], axis=1)
    S = np.concatenate([-np.sin(ar), np.sin(ar), -np.sin(ac), np.sin(ac)], axis=1)
    ropeC = np.ascontiguousarray(np.tile(C.reshape(16, 128, 1, 64), (1, 1, 10, 1)).reshape(16, 128, 640).astype(np.float32))
    ropeS = np.ascontiguousarray(np.tile(S.reshape(16, 128, 1, 64), (1, 1, 10, 1)).reshape(16, 128, 640).astype(np.float32))
    p = np.arange(128)[:, None]; f = np.arange(128)[None, :]
    cm = np.stack([(p == f), (p <= f), (p > f), (p >= f), (p < f), np.ones((128, 128), bool)], axis=1).astype(np.float32)
    return ropeC, ropeS, np.ascontiguousarray(cm.reshape(128, 6 * 128))


_NC_CACHE = {}


def _lay(v, k):
    return np.ascontiguousarray(v.reshape(k, 128).T)


def _prep(x, c, ctx, c_ctx, w_mod, b_mod, norm1, norm2, w_in, q_norm, k_norm, attn_sink,
          w_alpha_f, b_alpha_f, w_alpha_b, b_alpha_b, gla_norm, w_branch_attn, w_branch_gla,
          w_out, w_router, b_router, w_exp_in, b_exp_in, w_exp_out, b_exp_out):
    f = lambda a: np.ascontiguousarray(np.asarray(a, dtype=np.float32))
    x = f(x); c = f(c); ctx = f(ctx); c_ctx = f(c_ctx)
    ropeC, ropeS, cm = _host_consts()
    rep = lambda v: np.ascontiguousarray(np.broadcast_to(f(v).reshape(1, -1), (128, f(v).size)))
    bm = f(b_mod)[0]
    shared = {
        "w_mod": f(w_mod)[0], "b_modT": _lay(bm[0:2048], 16), "b_mod_b": rep(bm[2048:6144]),
        "norm1T": _lay(f(norm1)[0], 8), "norm2_b": rep(f(norm2)[0]),
        "w_in": f(w_in)[0], "qk_gain_b": rep(np.concatenate([f(q_norm)[0], f(k_norm)[0]])),
        "sink_b": rep(f(attn_sink)[0]),
        "w_alpha_f_aug": np.ascontiguousarray(np.concatenate([f(w_alpha_f)[0], f(b_alpha_f)[0][None, :], np.zeros((15, 256), np.float32)], axis=0)),
        "w_alpha_b_aug": np.ascontiguousarray(np.concatenate([f(w_alpha_b)[0], f(b_alpha_b)[0][None, :], np.zeros((15, 256), np.float32)], axis=0)),
        "gla_norm_b": rep(f(gla_norm)[0]),
        "w_branch_attn": f(w_branch_attn)[0], "w_branch_gla": f(w_branch_gla)[0], "w_out": f(w_out)[0],
        "w_router": f(w_router)[0], "b_router_b": rep(f(b_router)[0]),
        "w_exp_in": f(w_exp_in)[0],
        "bias1T": np.ascontiguousarray(f(b_exp_in)[0].reshape(N_EXP, 16, 128).transpose(2, 0, 1).reshape(128, N_EXP * 16)),
        "w_exp_out": f(w_exp_out)[0], "b_exp_out": f(b_exp_out)[0],
        "ropeC": ropeC, "ropeS": ropeS, "cmask": cm,
        "moe_c": np.ascontiguousarray(np.broadcast_to(np.concatenate([np.arange(32), np.arange(32) * CAP]).astype(np.float32)[None, :], (128, 64))),
        "hm": np.ascontiguousarray(np.stack([(np.arange(128) < 64), (np.arange(128) >= 64)], axis=1).astype(np.float32)),
    }
    in_maps = []
    for b in range(8):
        m = dict(shared)
        m["x"] = x[b]; m["ctx"] = ctx[b]
        m["cvec"] = np.ascontiguousarray(np.stack([_lay(c[b], 8), _lay(c_ctx, 8)], axis=2).reshape(128, 16))
        in_maps.append(m)
    return in_maps


def kernel(**inputs):
    if "nc" not in _NC_CACHE:
        _NC_CACHE["nc"] = build()
    nc = _NC_CACHE["nc"]
    in_maps = _prep(**inputs)
    res = run_bass_kernel_spmd(nc, in_maps, core_ids=list(range(8)))
    return np.stack([np.asarray(res.results[b]["out"], dtype=np.float32) for b in range(8)], axis=0)
```

```python
import numpy as np
from contextlib import ExitStack
import concourse.bass as bass
import concourse.mybir as mybir
from concourse.bass_utils import run_bass_kernel_spmd

F32 = mybir.dt.float32
BF16 = mybir.dt.bfloat16
U8 = mybir.dt.uint8
I32 = mybir.dt.int32
U32 = mybir.dt.uint32
CAP = 1024
TRASH = 32 * CAP
AF = mybir.ActivationFunctionType
ALU = mybir.AluOpType
AX = mybir.AxisListType

ENGS = ("pe", "act", "dve", "pool", "sp")
DEBUG = False
GLA_LEVEL = 9
GLA_TILES = 99
ACT_SEL = 15
OFFS = {}
N_EXP = 32


class Op:
    __slots__ = ("eng", "fn", "deps", "idx", "signal", "dma", "group", "gcount", "name", "scount")


class Prog:
    def __init__(self, nc, same_engine_sync=True):
        self.nc = nc
        self.ops = {e: [] for e in ENGS}
        self.last_write = {}
        self.readers = {}
        self.group_total = {}
        self.group_waitall = set()
        self.same_engine_sync = same_engine_sync
        self.out_groups = set()

    @staticmethod
    def _norm(keys):
        out = []
        for k in keys:
            if isinstance(k, str) and len(k) >= 2 and k[0] == "b" and k[1].isdigit():
                k = "bank" + k[1]
            out.append(k)
        return tuple(out)

    def _add(self, eng, fn, reads, writes, dma, group, name):
        reads = self._norm(reads); writes = self._norm(writes)
        writes = writes + tuple(k for k in reads if isinstance(k, str) and k.startswith("bank") and k not in writes)
        o = Op()
        o.eng = eng; o.fn = fn; o.deps = set(); o.signal = False
        o.dma = dma; o.group = group; o.gcount = 0; o.name = name; o.scount = None
        for k in reads:
            w = self.last_write.get(k)
            if w is not None:
                o.deps.add(w)
        for k in writes:
            w = self.last_write.get(k)
            if w is not None:
                o.deps.add(w)
            for r in self.readers.get(k, ()):
                o.deps.add(r)
        for k in writes:
            self.last_write[k] = o
            self.readers[k] = []
        for k in reads:
            self.readers.setdefault(k, []).append(o)
        o.deps.discard(o)
        o.idx = len(self.ops[eng])
        self.ops[eng].append(o)
        if dma:
            self.group_total[group] = self.group_total.get(group, 0) + 1
            o.gcount = self.group_total[group]
        return o

    def op(self, eng, fn, reads=(), writes=(), name=None):
        return self._add(eng, fn, tuple(reads), tuple(writes), False, None, name)

    def dma(self, eng, fn, reads=(), writes=(), group=None, waitall=False, is_output=False, name=None):
        assert group is not None
        if waitall:
            self.group_waitall.add(group)
        if is_output:
            self.out_groups.add(group)
        return self._add(eng, fn, tuple(reads), tuple(writes), True, group, name)

    def barrier(self):
        lasts = [self.ops[e][-1] for e in ENGS if self.ops[e]]
        dmas = {}
        for e in ENGS:
            for o in self.ops[e]:
                if o.dma:
                    dmas[o.group] = o
        deps = [d for d in lasts if d.fn is not None] + list(dmas.values())
        for e in ENGS:
            o = Op()
            o.eng = e; o.fn = None; o.deps = set(deps); o.signal = False
            o.dma = False; o.group = None; o.gcount = 0; o.name = "barrier"; o.scount = None
            o.idx = len(self.ops[e])
            self.ops[e].append(o)
        self.last_write = {}
        self.readers = {}

    def _same_skip(self, d, e):
        return d.eng == e and not (self.same_engine_sync and e in ("act", "dve", "pool"))

    def emit(self):
        nc = self.nc
        for e in ENGS:
            for o in self.ops[e]:
                for d in o.deps:
                    if not d.dma and d.fn is not None and not self._same_skip(d, e):
                        d.signal = True
        with ExitStack() as es:
            esem = {e: es.enter_context(nc.semaphore("s_" + e)) for e in ENGS}
            gsem = {g: es.enter_context(nc.semaphore("g_%d" % i))
                    for i, g in enumerate(sorted(self.group_total, key=str))}
            for e in ENGS:
                c = 0
                for o in self.ops[e]:
                    if o.signal and not o.dma:
                        c += 1
                    o.scount = c
            block = es.enter_context(nc.Block())

            def run(e, engobj):
                waited = {}
                for o in self.ops[e]:
                    need = {}
                    for d in o.deps:
                        if d.dma:
                            g = d.group
                            v = 16 * (self.group_total[g] if g in self.group_waitall else d.gcount)
                            key = ("g", g)
                        else:
                            if self._same_skip(d, e):
                                continue
                            if d.fn is None:
                                continue
                            key = ("e", d.eng)
                            v = d.scount
                        need[key] = max(need.get(key, 0), v)
                    for key, v in need.items():
                        if waited.get(key, 0) >= v or v == 0:
                            continue
                        waited[key] = v
                        sem = gsem[key[1]] if key[0] == "g" else esem[key[1]]
                        engobj.wait_ge(sem, v)
                    if o.fn is None:
                        continue
                    ins = o.fn(engobj)
                    if o.dma:
                        ins.then_inc(gsem[o.group], 16)
                    elif o.signal:
                        ins.then_inc(esem[e], 1)
                if e == "sp":
                    for g in sorted(self.out_groups, key=str):
                        engobj.wait_ge(gsem[g], 16 * self.group_total[g])

            block.tensor(lambda t: run("pe", t))
            block.scalar(lambda t: run("act", t))
            block.vector(lambda t: run("dve", t))
            block.gpsimd(lambda t: run("pool", t))
            block.sync(lambda t: run("sp", t))


class Arena:
    def __init__(self, ar, size):
        self.ar = ar; self.size = size; self.top = 0

    def alloc(self, shape, dt):
        nel = int(np.prod(shape))
        esz = 4 if dt in (F32, I32, U32) else 2
        nb = (nel * esz + 63) // 64 * 64
        assert self.top + nb <= self.size, ("arena overflow", self.top, nb, self.size)
        v = self.ar[:, self.top:self.top + nel * esz].bitcast(dt)
        self.top += nb
        if len(shape) == 2:
            v = v.rearrange("p (a b) -> p a b", a=shape[0])
        elif len(shape) == 3:
            v = v.rearrange("p (a b c) -> p a b c", a=shape[0], b=shape[1])
        return v

    def mark(self):
        return self.top

    def release(self, m):
        self.top = m


def build(dbg=False, n_exp_run=N_EXP, stop_after=99):
    nc = bass.Bass("TRN2", target_bir_lowering=False)
    T, LC, D = 2048, 256, 1024
    NT, NCT = 16, 2
    NTT = NT + NCT

    def din(name, shape, dt=F32):
        return nc.dram_tensor(name, list(shape), dt, kind="ExternalInput").ap()

    x_d = din("x", [T, D]); ctx_d = din("ctx", [LC, D]); cvec_d = din("cvec", [128, 16])
    wmod_d = din("w_mod", [D, 6144]); bmodT_d = din("b_modT", [128, 16]); bmodb_d = din("b_mod_b", [128, 4096])
    n1T_d = din("norm1T", [128, 8]); n2b_d = din("norm2_b", [128, D])
    win_d = din("w_in", [D, 4384]); qkg_d = din("qk_gain_b", [128, 128]); sink_d = din("sink_b", [128, 8])
    waf_d = din("w_alpha_f_aug", [32, 256]); wab_d = din("w_alpha_b_aug", [32, 256])
    gnorm_d = din("gla_norm_b", [128, 128])
    wba_d = din("w_branch_attn", [512, D]); wbg_d = din("w_branch_gla", [512, D]); wout_d = din("w_out", [D, D])
    wr_d = din("w_router", [D, 32]); brb_d = din("b_router_b", [128, 32])
    w1_d = din("w_exp_in", [N_EXP, D, 2048]); b1T_d = din("bias1T", [128, N_EXP * 16])
    w2_d = din("w_exp_out", [N_EXP, D, D]); b2_d = din("b_exp_out", [N_EXP, D])
    ropeC_d = din("ropeC", [NT, 128, 640]); ropeS_d = din("ropeS", [NT, 128, 640])
    hm_d = din("hm", [128, 2])
    moec_d = din("moe_c", [128, 64])
    RB = 32 * CAP + 128
    xs_d = nc.dram_tensor("xs_buf", [RB, D], BF16, kind="Internal").ap()
    ys_d = nc.dram_tensor("ys_buf", [RB, D], F32, kind="Internal").ap()
    cmask_d = din("cmask", [128, 6 * 128])
    out_d = nc.dram_tensor("out", [T, D], F32, kind="ExternalOutput").ap()
    x1_d = nc.dram_tensor("x1_buf", [T, D], F32, kind="Internal").ap()
    dbg_d = {}
    if dbg:
        for nm, shp in (("d_hT", [128, 8 * 2304]), ("d_attn_oT", [128, 4 * 2048]), ("d_oglaT", [128, 4 * 2048]),
                        ("d_yT", [128, 8 * 2048]), ("d_W", [128, 16 * 32]), ("d_h2T", [128, 8 * 2048]),
                        ("d_mod", [128, 4 * 1024 + 32]), ("d_QT", [128, 4 * 2048]), ("d_KT", [128, 2304])):
            dbg_d[nm] = nc.dram_tensor(nm, shp, F32, kind="ExternalOutput").ap()

    ARENA = 207 * 1024
    es = ExitStack()
    ar_t = es.enter_context(nc.sbuf_tensor("arena", [128, ARENA], U8))
    psum_t = es.enter_context(nc.psum_tensor("psum", [128, 4096], F32))
    A = Arena(ar_t, ARENA)
    P = Prog(nc)

    def finish():
        P.emit()
        es.close()
        return nc

    def bank(b):
        return psum_t[:, b * 512:(b + 1) * 512]

    def bank_bf(b):
        return psum_t[:, b * 512:(b + 1) * 512].bitcast(BF16)

    def MM(out, lhsT, rhs, start, stop, r, w):
        P.op("pe", lambda e: e.matmul(out, lhsT, rhs, start=start, stop=stop), r, w)

    def TR(out, in_, ident, r, w):
        P.op("pe", lambda e: e.transpose(out, in_, ident), r, w)

    def ACTV(out, in_, func, r, w, bias=None, scale=None, accum=None, eng="act"):
        kw = {}
        if bias is not None:
            kw["bias"] = bias
        if scale is not None:
            kw["scale"] = scale
        if accum is not None:
            kw["accum_out"] = accum
        P.op(eng, lambda e: e.activation(out=out, in_=in_, func=func, **kw), r, w)

    def TT(eng, out, in0, in1, op, r, w):
        P.op(eng, lambda e: e.tensor_tensor(out=out, in0=in0, in1=in1, op=op), r, w)

    def TS(eng, out, in0, s1, op0, r, w, s2=None, op1=None):
        if op1 is None:
            P.op(eng, lambda e: e.tensor_scalar(out=out, in0=in0, scalar1=s1, scalar2=None, op0=op0), r, w)
        else:
            P.op(eng, lambda e: e.tensor_scalar(out=out, in0=in0, scalar1=s1, scalar2=s2, op0=op0, op1=op1), r, w)

    def STT(eng, out, in0, scalar, in1, op0, op1, r, w):
        P.op(eng, lambda e: e.scalar_tensor_tensor(out=out, in0=in0, scalar=scalar, in1=in1, op0=op0, op1=op1), r, w)

    def CP(eng, out, in_, r, w):
        if eng == "act":
            P.op(eng, lambda e: e.copy(out=out, in_=in_), r, w)
        else:
            P.op(eng, lambda e: e.tensor_copy(out=out, in_=in_), r, w)

    def RED(out, in_, r, w):
        P.op("dve", lambda e: e.tensor_reduce(out=out, in_=in_, axis=AX.X, op=ALU.add), r, w)

    def RECIP(out, in_, r, w):
        P.op("dve", lambda e: e.reciprocal(out=out, in_=in_), r, w)

    def MEMSET(eng, ap, val, w):
        P.op(eng, lambda e: e.memset(ap, val), (), w)

    def DMA(eng, out, in_, r, w, group, waitall=False, is_output=False):
        P.dma(eng, lambda e: e.dma_start(out=out, in_=in_), r, w, group=group, waitall=waitall, is_output=is_output)

    def dump(name, src_ap, shape2, keys, dt):
        if not dbg:
            return
        m = A.mark()
        n = shape2
        done = 0
        tmp = A.alloc([256], F32)
        while done < n:
            c = min(256, n - done)
            CP("dve", tmp[:, 0:c], src_ap[:, done:done + c], list(keys) + ["dumptmp"], ["dumptmp"])
            DMA("sp", dbg_d[name][:, done:done + c], tmp[:, 0:c], ["dumptmp"], ["dumpdram" + name], "dbg_" + name, is_output=True)
            done += c
        A.release(m)

    NU = 6
    ring = A.alloc([NU, 8, 512], BF16)
    cm_f = A.alloc([6, 128], F32)
    cm_b = A.alloc([6, 128], BF16)
    gt2_b = A.alloc([1024], F32)
    W_all = A.alloc([NT, 32], F32)
    m4 = A.alloc([2, 4, 128], BF16)
    dest_i = A.alloc([NT, 4], I32)
    wsel = A.alloc([NT, 4], F32)
    moec = A.alloc([64], F32)
    small = A.alloc([512], F32)
    cvec = small[:, 0:16]; silu_c = small[:, 16:32]
    bmodT = small[:, 32:48]; n1T = small[:, 48:56]
    A1 = small[:, 56:72]
    B1 = small[:, 72:88]
    sinkb = small[:, 88:96]; esink = small[:, 96:104]
    brb = small[:, 104:136]
    modT = small[:, 136:168]
    qkg = small[:, 168:296]
    gnorm = small[:, 296:424]
    hm = small[:, 424:426]
    IDENT_F = cm_f[:, 0, :]; LE_F = cm_f[:, 1, :]; GT_F = cm_f[:, 2, :]; GE_F = cm_f[:, 3, :]; LT_F = cm_f[:, 4, :]; ONES_F = cm_f[:, 5, :]
    IDENT_B = cm_b[:, 0, :]; LE_B = cm_b[:, 1, :]; GE_B = cm_b[:, 3, :]; ONES_B = cm_b[:, 5, :]

    ring_use = [0]

    def load_unit(src_ap, shape3=None, ncols=512):
        s = ring_use[0] % NU
        ring_use[0] += 1
        key = "ring%d" % s
        if shape3 is not None:
            dst = ring[:, s, :, :].rearrange("p a b -> p (a b)").rearrange("p (a b) -> p a b", a=shape3[0])
        else:
            dst = ring[:, s, :, 0:ncols]
        DMA("pool", dst, src_ap, [], [key], "g" + key)
        return dst, key

    def wsrc(d_ap, c0, ncols=512):
        return d_ap[:, c0:c0 + ncols].rearrange("(k p) n -> p k n", p=128)

    DMA("sp", cvec, cvec_d, [], ["cvec"], "const", waitall=True)
    DMA("sp", bmodT, bmodT_d, [], ["bmodT"], "const", waitall=True)
    DMA("sp", n1T, n1T_d, [], ["n1T"], "const", waitall=True)
    DMA("sp", sinkb, sink_d, [], ["sinkb"], "const", waitall=True)
    DMA("sp", brb, brb_d, [], ["brb"], "const", waitall=True)
    DMA("sp", qkg, qkg_d, [], ["qkg"], "const", waitall=True)
    DMA("sp", gnorm, gnorm_d, [], ["gnorm"], "const", waitall=True)
    DMA("sp", hm, hm_d, [], ["hm"], "const", waitall=True)
    DMA("sp", moec, moec_d, [], ["moec"], "const", waitall=True)
    DMA("sp", cm_f.rearrange("p a b -> p (a b)"), cmask_d, [], ["cm_f"], "const", waitall=True)
    CP("dve", cm_b.rearrange("p a b -> p (a b)"), cm_f.rearrange("p a b -> p (a b)"), ["cm_f"], ["cm_b"])
    for i4 in range(4):
        CP("dve", m4[:, 0, i4, :], cm_b[:, 1, :], ["cm_b"], ["m4"])
        CP("dve", m4[:, 1, i4, :], cm_b[:, 3, :], ["cm_b"], ["m4"])
    ACTV(silu_c, cvec, AF.Silu, ["cvec"], ["silu_c"])
    ACTV(esink, sinkb, AF.Exp, ["sinkb"], ["esink"])

    m0 = A.mark()
    modvec = A.alloc([3, 1024], F32)
    gt1_b = modvec[:, 0, :]; A2_b = modvec[:, 1, :]; B2_b = modvec[:, 2, :]
    m0b = A.mark()
    wst = A.alloc([2, 8, 512], F32)
    silu_bc = A.alloc([8, 128], F32)
    bias_t = A.alloc([2, 512], F32)
    n2b = A.alloc([1024], F32)
    DMA("sp", n2b, n2b_d, [], ["n2b"], "const", waitall=True)
    sview = silu_c.rearrange("p (k n) -> p k n", n=2)
    for kc in range(8):
        CP("dve", silu_bc[:, kc, :], sview[:, kc, 0:1].to_broadcast([128, 128]), ["silu_c"], ["silu_bc"])
    for u in range(4):
        s = u % 2
        DMA("sp", wst[:, s, :, :], wsrc(wmod_d, u * 512), [], ["wst%d" % s], "gwst%d" % s)
        for j in range(4):
            jj = u * 4 + j
            for kc in range(8):
                MM(bank(2)[:, jj * 2:jj * 2 + 2], wst[:, s, kc, j * 128:(j + 1) * 128], sview[:, kc, :],
                   kc == 0, kc == 7, ["wst%d" % s, "silu_c"], ["b2mod"])
    bmv = bmodT.unsqueeze(2).to_broadcast([128, 16, 2])
    TT("dve", modT.rearrange("p (j n) -> p j n", n=2), bank(2)[:, 0:32].rearrange("p (j n) -> p j n", n=2), bmv, ALU.add,
       ["b2mod", "bmodT"], ["modT"])
    mT = modT.rearrange("p (j n) -> p j n", n=2)
    for n in range(2):
        STT("dve", A1[:, n * 8:(n + 1) * 8], mT[:, 8:16, n], 1.0, n1T, ALU.add, ALU.mult, ["modT", "n1T"], ["A1_%d" % n])
        CP("dve", B1[:, n * 8:(n + 1) * 8], mT[:, 0:8, n], ["modT"], ["B1_%d" % n])
    for u in range(8):
        s = u % 2
        c0 = 2048 + u * 512
        DMA("sp", wst[:, s, :, :], wsrc(wmod_d, c0), [], ["wst%d" % s], "gwst%d" % s)
        DMA("sp", bias_t[:, s, :], bmodb_d[:, u * 512:(u + 1) * 512], [], ["bias%d" % s], "gbias%d" % s)
        pb = bank(s)
        for kc in range(8):
            MM(pb, silu_bc[:, kc, :], wst[:, s, kc, :], kc == 0, kc == 7, ["wst%d" % s, "silu_bc"], ["pb%d" % s])
        vec = u // 2
        half = (u % 2) * 512
        if vec == 0:
            TT("dve", gt1_b[:, half:half + 512], pb, bias_t[:, s, :], ALU.add, ["pb%d" % s, "bias%d" % s], ["gt1_b%d" % half])
        elif vec == 1:
            TT("dve", B2_b[:, half:half + 512], pb, bias_t[:, s, :], ALU.add, ["pb%d" % s, "bias%d" % s], ["B2_b%d" % half])
        elif vec == 2:
            TT("dve", A2_b[:, half:half + 512], pb, bias_t[:, s, :], ALU.add, ["pb%d" % s, "bias%d" % s], ["A2_t%d" % half])
            STT("dve", A2_b[:, half:half + 512], A2_b[:, half:half + 512], 1.0, n2b[:, half:half + 512], ALU.add, ALU.mult,
                ["A2_t%d" % half, "n2b"], ["A2_b%d" % half])
        else:
            TT("dve", gt2_b[:, half:half + 512], pb, bias_t[:, s, :], ALU.add, ["pb%d" % s, "bias%d" % s], ["gt2_b%d" % half])
    if dbg:
        dump("d_mod", modvec.rearrange("p a b -> p (a b)"), 3072, ["gt1_b0", "gt1_b512", "A2_b0", "A2_b512", "B2_b0", "B2_b512"], F32)
    P.barrier()
    A.release(m0b)

    OFFS['hT'] = A.top
    hT = A.alloc([8, LC + T], BF16)
    m1 = A.mark()
    xt = A.alloc([2, 1024], F32)
    xn = A.alloc([2, 1024], BF16)
    junk = A.alloc([1024], BF16)
    st1 = A.alloc([NTT, 4], F32)
    htmp = A.alloc([8, 128], F32)
    for ti in range(NTT):
        s = ti % 2
        src = ctx_d[ti * 128:(ti + 1) * 128, :] if ti < NCT else x_d[(ti - NCT) * 128:(ti - NCT + 1) * 128, :]
        isctx = 1 if ti < NCT else 0
        DMA("sp", xt[:, s, :], src, [], ["xt%d" % s], "gxt%d" % s)
        ACTV(junk, xt[:, s, :], AF.Square, ["xt%d" % s], ["junk", "ssq%d" % s], accum=st1[:, ti, 0:1])
        ACTV(st1[:, ti, 1:2], st1[:, ti, 0:1], AF.Sqrt, ["ssq%d" % s], ["rms%d" % s], scale=1.0 / D, bias=1e-6)
        RECIP(st1[:, ti, 2:3], st1[:, ti, 1:2], ["rms%d" % s], ["rstd%d" % s])
        TS("dve", xn[:, s, :], xt[:, s, :], st1[:, ti, 2:3], ALU.mult, ["xt%d" % s, "rstd%d" % s], ["xn%d" % s])
        pb = bank_bf(s)
        for kc in range(8):
            TR(pb[:, kc * 128:(kc + 1) * 128], xn[:, s, kc * 128:(kc + 1) * 128], IDENT_B, ["xn%d" % s, "cm_b"], ["ptr%d" % s])
        a_bc = A1[:, isctx * 8:(isctx + 1) * 8].unsqueeze(2).to_broadcast([128, 8, 128])
        b_bc = B1[:, isctx * 8:(isctx + 1) * 8].unsqueeze(2).to_broadcast([128, 8, 128])
        hdst = hT[:, :, ti * 128:(ti + 1) * 128]
        TT("dve", htmp, pb.rearrange("p (k t) -> p k t", k=8), a_bc, ALU.mult, ["ptr%d" % s, "A1_%d" % isctx], ["htmp"])
        TT("dve", hdst, htmp, b_bc, ALU.add, ["htmp", "B1_%d" % isctx], ["hT%d" % ti])
    if dbg:
        dump("d_hT", hT.rearrange("p a b -> p (a b)"), 8 * 2304, ["hT%d" % t for t in range(NTT)], BF16)
    P.barrier()
    A.release(m1)
    if stop_after <= 1:
        return finish()

    mA = A.mark()
    o_glaT = A.alloc([4, T], BF16)
    attn_oT = A.alloc([4, T], BF16)

    m2 = A.mark()
    o_f = A.alloc([NT, 512], BF16)
    gr_s = A.alloc([NT, 512], BF16)
    wlr = A.alloc([8, 32], BF16)
    walpha = A.alloc([2, 256], BF16)
    lrT = A.alloc([128], BF16)
    la = A.alloc([256], F32)
    la_hi = A.alloc([256], BF16); la_lo = A.alloc([256], BF16)
    Eq = A.alloc([256], F32); Ek = A.alloc([256], F32); Eh = A.alloc([256], F32)
    qt_ = A.alloc([256], BF16); kt_ = A.alloc([256], BF16); kh_ = A.alloc([256], BF16)
    v_bf = A.alloc([512], BF16)
    qkT = A.alloc([4, 128], BF16)
    AT = A.alloc([4, 128], BF16)
    qpad = A.alloc([4, 128], BF16)
    S32 = A.alloc([2, 128], F32)
    S_bf = A.alloc([2, 128], BF16)
    gam = A.alloc([4], F32)
    osum = A.alloc([512], F32)
    osq = A.alloc([512], F32)
    ost = A.alloc([16], F32)
    on_bf = A.alloc([512], BF16)
    gnorm4 = A.alloc([4, 128], F32)
    DMA("pool", wlr, win_d[:, 2304:2336].rearrange("(k p) n -> p k n", p=128), [], ["wlr"], "const2", waitall=True)
    DMA("pool", walpha[0:32, 0, :], waf_d, [], ["walpha0"], "const2", waitall=True)
    DMA("pool", walpha[0:32, 1, :], wab_d, [], ["walpha1"], "const2", waitall=True)
    MEMSET("dve", lrT[0:32, :], 1.0, ["lrT"])
    for i4 in range(4):
        CP("dve", gnorm4[:, i4, :], gnorm, ["gnorm"], ["gnorm4"])
    u_qk, k_qk = load_unit(wsrc(win_d, 768))
    u_v, k_v = load_unit(wsrc(win_d, 1280))
    u_r, k_r = load_unit(wsrc(win_d, 1792))

    def gla_pass(direction):
        fwd = direction == 0
        MEMSET("dve", S32.rearrange("p a b -> p (a b)"), 0.0, ["S32"])
        MEMSET("dve", S_bf.rearrange("p a b -> p (a b)"), 0.0, ["S_bf"])
        if fwd:
            order = list(range(NTT))
        else:
            order = [1, 0] + [NCT + n for n in range(NT - 1, -1, -1)]
        MP = cm_b[:, 1, :] if fwd else cm_b[:, 3, :]
        MD = cm_b[:, 2, :] if fwd else cm_b[:, 4, :]
        MA = m4[:, 0, :, :] if fwd else m4[:, 1, :, :]
        lc0 = 0 if fwd else 16
        for ti in order[:GLA_TILES]:
            isctx = ti < NCT
            n = ti - NCT
            hcol = slice(ti * 128, (ti + 1) * 128)
            hk = "hT%d" % ti
            for kc in range(8):
                MM(bank(0), hT[:, kc, hcol], u_qk[:, kc, :], kc == 0, kc == 7, [k_qk], ["b0"])
            for kc in range(8):
                MM(bank(1), hT[:, kc, hcol], u_v[:, kc, :], kc == 0, kc == 7, [k_v], ["b1"])
            if fwd and not isctx:
                for kc in range(8):
                    MM(bank(2), hT[:, kc, hcol], u_r[:, kc, :], kc == 0, kc == 7, [k_r], ["b2"])
                ACTV(gr_s[:, n, :], bank(2), AF.Silu, ["b2"], ["gr_s%d" % n])
            for kc in range(8):
                MM(bank(3)[0:16, 0:128], wlr[:, kc, lc0:lc0 + 16], hT[:, kc, hcol], kc == 0, kc == 7, ["wlr"], ["b3a"])
            CP("dve", lrT[0:16, :], bank(3)[0:16, 0:128], ["b3a"], ["lrT"])
            CP("act", v_bf, bank(1), ["b1"], ["v_bf"])
            if GLA_LEVEL <= 0:
                continue
            MM(bank(3)[:, 256:512], lrT[0:32, :], walpha[0:32, direction, :], True, True, ["lrT", "walpha%d" % direction], ["b3z"])
            ACTV(la, bank(3)[:, 256:512], AF.Exp, ["b3z"], ["la_e", "la"], scale=-1.0)
            ACTV(la, la, AF.Ln, ["la_e"], ["la"], bias=1.0)
            if GLA_LEVEL <= 1:
                continue
            CP("act", la_hi, la, ["la"], ["la_hi"])
            TT("dve", la_lo, la, la_hi, ALU.subtract, ["la", "la_hi"], ["la_lo"])
            MM(bank(4)[:, 0:256], MP, la_hi, True, False, ["la_hi", "cm_b"], ["b4p"])
            MM(bank(4)[:, 0:256], MP, la_lo, False, True, ["la_lo", "cm_b"], ["b4p"])
            MM(bank(4)[:, 256:512], MD, la_hi, True, False, ["la_hi", "cm_b"], ["b4d"])
            MM(bank(4)[:, 256:512], MD, la_lo, False, True, ["la_lo", "cm_b"], ["b4d"])
            if GLA_LEVEL <= 1.3:
                continue
            for pr in range(2):
                MM(bank(5)[:, 256 + 2 * pr:258 + 2 * pr], la_hi[:, pr * 128:(pr + 1) * 128], ONES_B[:, 0:2], True, False, ["la_hi", "cm_b"], ["b5g%d" % pr])
                MM(bank(5)[:, 256 + 2 * pr:258 + 2 * pr], la_lo[:, pr * 128:(pr + 1) * 128], ONES_B[:, 0:2], False, True, ["la_lo", "cm_b"], ["b5g%d" % pr])
            if GLA_LEVEL <= 1.6:
                continue
            if ACT_SEL & 1:
                ACTV(Eq, bank(4)[:, 0:256], AF.Exp, ["b4p"], ["Eq"], scale=-1.0 / 16)
            if ACT_SEL & 2:
                ACTV(Ek, bank(4)[:, 0:256], AF.Exp, ["b4p"], ["Ek"], scale=1.0 / 16)
            if ACT_SEL & 4:
                ACTV(Eh, bank(4)[:, 256:512], AF.Exp, ["b4d"], ["Eh"], scale=-1.0 / 16)
            if ACT_SEL & 8:
                ACTV(gam[:, 0:4], bank(5)[:, 256:260], AF.Exp, ["b5g0", "b5g1"], ["gam"], scale=-1.0 / 16)
            if GLA_LEVEL <= 2:
                continue
            if not isctx:
                STT("dve", qt_, bank(0)[:, 0:256], 0.125, Eq, ALU.mult, ALU.mult, ["b0", "Eq"], ["qt_"])
                TT("dve", kt_, bank(0)[:, 256:512], Ek, ALU.mult, ["b0", "Ek"], ["kt_"])
            TT("dve", kh_, bank(0)[:, 256:512], Eh, ALU.mult, ["b0", "Eh"], ["kh_"])
            if not isctx:
                pbt = bank_bf(5)
                for pr in range(2):
                    TR(pbt[:, pr * 128:(pr + 1) * 128], qt_[:, pr * 128:(pr + 1) * 128], IDENT_B, ["qt_", "cm_b"], ["b5t"])
                    TR(pbt[:, (2 + pr) * 128:(3 + pr) * 128], kt_[:, pr * 128:(pr + 1) * 128], IDENT_B, ["kt_", "cm_b"], ["b5t"])
                CP("act", qkT.rearrange("p a b -> p (a b)"), pbt[:, 0:512], ["b5t"], ["qkT"])
                if GLA_LEVEL <= 2.4:
                    continue
                qp4 = qpad.rearrange("p (pr two) t -> p pr two t", two=2)
                for half in range(2):
                    TS("dve", qp4[:, :, half, :], pbt[:, 0:256].rearrange("p (pr t) -> p pr t", pr=2), hm[:, half:half + 1], ALU.mult,
                       ["b5t", "hm"], ["qpad"])
                if GLA_LEVEL <= 2.45:
                    continue
                for h in range(4):
                    MM(bank(6)[:, h * 128:(h + 1) * 128], qkT[:, 2 + h // 2, :], qpad[:, h, :], True, True, ["qkT", "qpad"], ["b6"])
                if GLA_LEVEL <= 2.5:
                    continue
                TT("dve", AT, bank(6).rearrange("p (h i) -> p h i", h=4), MA, ALU.mult, ["b6", "m4"], ["AT"])
                if GLA_LEVEL <= 2.6:
                    continue
                for h in range(4):
                    MM(bank(7)[:, h * 128:(h + 1) * 128], AT[:, h, :], v_bf[:, h * 128:(h + 1) * 128], True, False, ["AT", "v_bf"], ["b7"])
                    MM(bank(7)[:, h * 128:(h + 1) * 128], qpad[:, h, :], S_bf[:, h // 2, :], False, True, ["qpad", "S_bf"], ["b7"])
                if fwd:
                    CP("act", o_f[:, n, :], bank(7), ["b7"], ["o_f%d" % n])
                else:
                    TT("dve", osum, bank(7), o_f[:, n, :], ALU.add, ["b7", "o_f%d" % n], ["osum"])
                    TT("dve", osq, osum, osum, ALU.mult, ["osum"], ["osq"])
                    RED(ost[:, 0:4], osq.rearrange("p (h d) -> p h d", h=4), ["osq"], ["ost0"])
                    ACTV(ost[:, 4:8], ost[:, 0:4], AF.Sqrt, ["ost0"], ["ost1"], scale=1.0 / 128, bias=1e-6)
                    RECIP(ost[:, 8:12], ost[:, 4:8], ["ost1"], ["ost2"])
                    o3 = osum.rearrange("p (h d) -> p h d", h=4)
                    TT("dve", o3, o3, ost[:, 8:12].unsqueeze(2).to_broadcast([128, 4, 128]), ALU.mult, ["osum", "ost2"], ["osum"])
                    TT("dve", o3, o3, gnorm4, ALU.mult, ["osum", "gnorm4"], ["osum"])
                    TT("dve", on_bf, osum, gr_s[:, n, :], ALU.mult, ["osum", "gr_s%d" % n], ["on_bf"])
                    pbo = bank_bf(6)
                    for k4 in range(4):
                        TR(pbo[:, k4 * 128:(k4 + 1) * 128], on_bf[:, k4 * 128:(k4 + 1) * 128], IDENT_B, ["on_bf", "cm_b"], ["b6"])
                    CP("act", o_glaT[:, :, n * 128:(n + 1) * 128], pbo[:, 0:512].rearrange("p (k t) -> p k t", k=4), ["b6"], ["o_glaT%d" % n])
            if GLA_LEVEL <= 3:
                continue
            for pr in range(2):
                MM(bank(1)[:, pr * 256:(pr + 1) * 256], kh_[:, pr * 128:(pr + 1) * 128], v_bf[:, pr * 256:(pr + 1) * 256], True, True,
                   ["kh_", "v_bf"], ["b1"])
            for pr in range(2):
                TS("dve", S32[:, pr, :], S32[:, pr, :], gam[:, 2 * pr:2 * pr + 1], ALU.mult, ["S32", "gam"], ["S32"])
                for half in range(2):
                    c0 = pr * 256 + half * 128
                    STT("dve", S32[:, pr, :], bank(1)[:, c0:c0 + 128], hm[:, half:half + 1], S32[:, pr, :], ALU.mult, ALU.add,
                        ["S32", "hm", "b1"], ["S32"])
            CP("act", S_bf.rearrange("p a b -> p (a b)"), S32.rearrange("p a b -> p (a b)"), ["S32"], ["S_bf"])

    gla_pass(0)
    gla_pass(1)
    if dbg:
        dump("d_oglaT", o_glaT.rearrange("p a b -> p (a b)"), 4 * 2048, ["o_glaT%d" % t for t in range(NT)], BF16)
    P.barrier()
    A.release(m2)
    if stop_after <= 2:
        return finish()

    m3 = A.mark()
    QT = A.alloc([4, T], BF16)
    KT = A.alloc([2, LC + T], BF16)
    Vaug = A.alloc([NTT, 2, 66], BF16)
    ropeC = A.alloc([2, 640], F32); ropeS = A.alloc([2, 640], F32)
    qg10 = A.alloc([10, 64], F32)
    qst = A.alloc([40], F32)
    qn = A.alloc([640], F32)
    t1 = A.alloc([640], F32)
    t2 = A.alloc([640], F32)
    sq = t2
    qr_bf = A.alloc([640], BF16)
    PT = A.alloc([2, 5, 512], BF16)
    ao = A.alloc([512], BF16)
    den = A.alloc([8], F32)
    for i10 in range(10):
        CP("dve", qg10[:, i10, :], qkg[:, 0:64] if i10 < 8 else qkg[:, 64:128], ["qkg"], ["qg10"])
    MEMSET("dve", Vaug.rearrange("p a b c -> p (a b c)"), 1.0, ["Vaug"])
    u_q, k_q = load_unit(wsrc(win_d, 0))
    u_kv, k_kv = load_unit(wsrc(win_d, 512, 256), ncols=256)
    for ti in range(NTT):
        isctx = ti < NCT
        n = ti - NCT
        hcol = slice(ti * 128, (ti + 1) * 128)
        for kc in range(8):
            MM(bank(1)[:, 0:256], hT[:, kc, hcol], u_kv[:, kc, 0:256], kc == 0, kc == 7, [k_kv], ["b1"])
        if not isctx:
            for kc in range(8):
                MM(bank(0), hT[:, kc, hcol], u_q[:, kc, :], kc == 0, kc == 7, [k_q], ["b0"])
        CP("act", Vaug[:, ti, :, 0:64], bank(1)[:, 128:256].rearrange("p (k d) -> p k d", k=2), ["b1", "Vaug"], ["Vaug%d" % ti])
        nh = 2 if isctx else 10
        c_lo = 512 if isctx else 0
        if not isctx:
            ACTV(sq[:, 0:512], bank(0), AF.Square, ["b0"], ["sq", "t2a", "t2b"])
        ACTV(sq[:, 512:640], bank(1)[:, 0:128], AF.Square, ["b1"], ["sq", "t2a", "t2b"])
        hs = slice(c_lo // 64, 10)
        RED(qst[:, hs], sq[:, c_lo:640].rearrange("p (h d) -> p h d", d=64), ["sq"], ["qst0"])
        ACTV(qst[:, 10 + c_lo // 64:20], qst[:, hs], AF.Sqrt, ["qst0"], ["qst1"], scale=1.0 / 64, bias=1e-6)
        RECIP(qst[:, 20 + c_lo // 64:30], qst[:, 10 + c_lo // 64:20], ["qst1"], ["qst2"])
        if not isctx:
            TT("dve", qn[:, 0:512].rearrange("p (h d) -> p h d", d=64), bank(0).rearrange("p (h d) -> p h d", d=64),
               qst[:, 20:28].unsqueeze(2).to_broadcast([128, 8, 64]), ALU.mult, ["b0", "qst2"], ["qn_q"])
            TT("dve", qn[:, 0:512].rearrange("p (h d) -> p h d", d=64), qn[:, 0:512].rearrange("p (h d) -> p h d", d=64),
               qg10[:, 0:8, :], ALU.mult, ["qn_q", "qg10"], ["qn_q"])
        TT("dve", qn[:, 512:640].rearrange("p (h d) -> p h d", d=64), bank(1)[:, 0:128].rearrange("p (h d) -> p h d", d=64),
           qst[:, 28:30].unsqueeze(2).to_broadcast([128, 2, 64]), ALU.mult, ["b1", "qst2"], ["qn_k"])
        if isctx:
            TT("dve", qr_bf[:, 512:640].rearrange("p (h d) -> p h d", d=64), qn[:, 512:640].rearrange("p (h d) -> p h d", d=64),
               qg10[:, 8:10, :], ALU.mult, ["qn_k", "qg10"], ["qr_k"])
        else:
            TT("dve", qn[:, 512:640].rearrange("p (h d) -> p h d", d=64), qn[:, 512:640].rearrange("p (h d) -> p h d", d=64),
               qg10[:, 8:10, :], ALU.mult, ["qn_k", "qg10"], ["qn_k"])
            rs = n % 2
            DMA("sp", ropeC[:, rs, :], ropeC_d[n], [], ["ropeC%d" % rs], "gropeC%d" % rs)
            DMA("sp", ropeS[:, rs, :], ropeS_d[n], [], ["ropeS%d" % rs], "gropeS%d" % rs)
            TT("dve", t1, qn, ropeC[:, rs, :], ALU.mult, ["qn_q", "qn_k", "ropeC%d" % rs], ["t1"])
            q5 = qn.rearrange("p (h g two s) -> p h g two s", g=2, two=2, s=16)
            t5 = t2.rearrange("p (h g two s) -> p h g two s", g=2, two=2, s=16)
            S5 = ropeS[:, rs, :].rearrange("p (h g two s) -> p h g two s", g=2, two=2, s=16)
            TT("dve", t5[:, :, :, 0, :], q5[:, :, :, 1, :], S5[:, :, :, 0, :], ALU.mult,
               ["qn_q", "qn_k", "ropeS%d" % rs, "sq"], ["t2a", "sq"])
            TT("dve", t5[:, :, :, 1, :], q5[:, :, :, 0, :], S5[:, :, :, 1, :], ALU.mult,
               ["qn_q", "qn_k", "ropeS%d" % rs, "sq"], ["t2b", "sq"])
            TT("dve", qr_bf[:, 0:512].rearrange("p (g kv d) -> p kv g d", g=4, kv=2), t1[:, 0:512].rearrange("p (kv g d) -> p kv g d", kv=2, g=4),
               t2[:, 0:512].rearrange("p (kv g d) -> p kv g d", kv=2, g=4), ALU.add, ["t1", "t2a", "t2b"], ["qr_q"])
            TT("dve", qr_bf[:, 512:640], t1[:, 512:640], t2[:, 512:640], ALU.add, ["t1", "t2a", "t2b"], ["qr_k"])
        pbt = bank_bf(2 + ti % 2)
        pk = "b%dt" % (2 + ti % 2)
        TR(pbt[:, 512:640], qr_bf[:, 512:640], IDENT_B, ["qr_k", "cm_b"], [pk])
        if not isctx:
            for j in range(4):
                TR(pbt[:, j * 128:(j + 1) * 128], qr_bf[:, j * 128:(j + 1) * 128], IDENT_B, ["qr_q", "cm_b"], [pk])
            CP("act", QT[:, :, n * 128:(n + 1) * 128], pbt[:, 0:512].rearrange("p (j t) -> p j t", j=4), [pk], ["QT%d" % n])
        TS("dve", KT[:, 0, hcol], pbt[:, 512:640], hm[:, 0:1], ALU.mult, [pk, "hm"], ["KT%d" % ti])
        TS("dve", KT[:, 1, hcol], pbt[:, 512:640], hm[:, 1:2], ALU.mult, [pk, "hm", "KT%d" % ti], ["KT%d" % ti])
    if dbg:
        dump("d_QT", QT.rearrange("p a b -> p (a b)"), 4 * 2048, ["QT%d" % t for t in range(NT)], BF16)
        dump("d_KT", KT[:, 0, :], 2304, ["KT%d" % t for t in range(NTT)], BF16)
    it = 0
    for n in range(NT):
        keyt = [(0, None), (1, None)]
        if n > 0:
            keyt.append((NCT + n - 1, m4[:, 1, :, :]))
        keyt.append((NCT + n, None))
        if n < NT - 1:
            keyt.append((NCT + n + 1, m4[:, 0, :, :]))
        for kv in range(2):
            ps_ = slice(kv * 64, kv * 64 + 64)
            sl = it % 2
            it += 1
            for si, (kt, msk) in enumerate(keyt):
                b = 4 + (si % 3)
                MM(bank(b), KT[:, kv, kt * 128:(kt + 1) * 128], QT[:, :, n * 128:(n + 1) * 128], True, True,
                   ["KT%d" % kt, "QT%d" % n], ["b%d" % b])
                ACTV(PT[:, sl, si, :], bank(b), AF.Exp, ["b%d" % b], ["PT%d_%d" % (sl, si)], scale=0.125)
                if msk is not None:
                    p3 = PT[:, sl, si, :].rearrange("p (g q) -> p g q", g=4)
                    TT("dve", p3, p3, msk, ALU.mult, ["PT%d_%d" % (sl, si), "m4"], ["PT%d_%d" % (sl, si)])
            pvb = (7, 0)[it % 2]
            pob = bank(pvb)
            for g in range(4):
                for si, (kt, msk) in enumerate(keyt):
                    MM(pob[:, g * 66:g * 66 + 65], PT[:, sl, si, g * 128:(g + 1) * 128], Vaug[:, kt, kv, 0:65],
                       si == 0, si == len(keyt) - 1, ["PT%d_%d" % (sl, si), "Vaug%d" % kt], ["b%d" % pvb])
            po3 = pob[:, 0:264].rearrange("p (g c) -> p g c", g=4)
            TT("dve", den[:, 0:4], po3[:, :, 64], esink[:, kv * 4:(kv + 1) * 4], ALU.add, ["b%d" % pvb, "esink"], ["den0"])
            RECIP(den[:, 4:8], den[:, 0:4], ["den0"], ["den1"])
            TT("dve", ao[:, kv * 256:(kv + 1) * 256].rearrange("p (g d) -> p g d", g=4), po3[:, :, 0:64],
               den[:, 4:8].unsqueeze(2).to_broadcast([128, 4, 64]), ALU.mult, ["b%d" % pvb, "den1"], ["ao%d" % kv])
        pbo = bank_bf(3)
        for k4 in range(4):
            TR(pbo[:, k4 * 128:(k4 + 1) * 128], ao[:, k4 * 128:(k4 + 1) * 128], IDENT_B, ["ao0", "ao1", "cm_b"], ["b3t"])
        CP("act", attn_oT[:, :, n * 128:(n + 1) * 128], pbo[:, 0:512].rearrange("p (k t) -> p k t", k=4), ["b3t"], ["attn_oT%d" % n])
    if dbg:
        dump("d_attn_oT", attn_oT.rearrange("p a b -> p (a b)"), 4 * 2048, ["attn_oT%d" % t for t in range(NT)], BF16)
    P.barrier()
    A.release(m3)
    if stop_after <= 3:
        return finish()

    yT_start = A.mark()
    yT = A.alloc([8, T], BF16)
    m4 = A.mark()
    sga = A.alloc([2, 512], F32); sgg = A.alloc([2, 512], F32)
    ty1 = A.alloc([2, 512], F32); ty2 = A.alloc([2, 512], F32)
    u_ba, k_ba = load_unit(wba_d.rearrange("(k p) n -> p k n", p=128), shape3=[4, 1024])
    u_bg, k_bg = load_unit(wbg_d.rearrange("(k p) n -> p k n", p=128), shape3=[4, 1024])
    cnt = 0
    for dc in range(8):
        if dc % 4 == 0:
            u_ga, k_ga = load_unit(wsrc(win_d, 2336 + (dc // 4) * 512))
            u_gg, k_gg = load_unit(wsrc(win_d, 3360 + (dc // 4) * 512))
        mcol = slice((dc % 4) * 128, (dc % 4) * 128 + 128)
        for tb in range(4):
            s = cnt % 2
            cnt += 1
            tcol = slice(LC + tb * 512, LC + (tb + 1) * 512)
            ocol = slice(tb * 512, (tb + 1) * 512)
            hk = ["hT%d" % t for t in range(NCT + tb * 4, NCT + tb * 4 + 4)]
            b_ga, b_gg, b_p1, b_p2 = (0, 1, 2, 3) if s == 0 else (4, 5, 6, 7)
            for kc in range(8):
                MM(bank(b_ga), u_ga[:, kc, mcol], hT[:, kc, tcol], kc == 0, kc == 7, [k_ga], ["b%d" % b_ga])
            for kc in range(8):
                MM(bank(b_gg), u_gg[:, kc, mcol], hT[:, kc, tcol], kc == 0, kc == 7, [k_gg], ["b%d" % b_gg])
            for k4 in range(4):
                MM(bank(b_p1), u_ba[:, k4, dc * 128:(dc + 1) * 128], attn_oT[:, k4, ocol], k4 == 0, k4 == 3, [k_ba], ["b%d" % b_p1])
            for k4 in range(4):
                MM(bank(b_p2), u_bg[:, k4, dc * 128:(dc + 1) * 128], o_glaT[:, k4, ocol], k4 == 0, k4 == 3, [k_bg], ["b%d" % b_p2])
            ACTV(sga[:, s, :], bank(b_ga), AF.Sigmoid, ["b%d" % b_ga], ["sga%d" % s])
            ACTV(sgg[:, s, :], bank(b_gg), AF.Sigmoid, ["b%d" % b_gg], ["sgg%d" % s])
            TT("dve", ty1[:, s, :], bank(b_p1), sga[:, s, :], ALU.mult, ["b%d" % b_p1, "sga%d" % s], ["ty1%d" % s])
            TT("dve", ty2[:, s, :], bank(b_p2), sgg[:, s, :], ALU.mult, ["b%d" % b_p2, "sgg%d" % s], ["ty2%d" % s])
            TT("pool", yT[:, dc, ocol], ty1[:, s, :], ty2[:, s, :], ALU.add, ["ty1%d" % s, "ty2%d" % s], ["yT"])
    if dbg:
        dump("d_yT", yT.rearrange("p a b -> p (a b)"), 8 * 2048, ["yT"], BF16)
    P.barrier()
    A.release(m4)
    if stop_after <= 4:
        return finish()

    h2T = hT[:, :, 0:T]
    m5 = A.mark()
    A.release(mA)
    xt2 = A.alloc([2, 1024], F32)
    x1 = A.alloc([2, 1024], F32)
    h2 = A.alloc([1, 1024], F32)
    h2hi = A.alloc([1024], BF16); h2lo = A.alloc([1024], BF16)
    h2Tlo = A.alloc([8, 128], BF16)
    wr_f = A.alloc([8, 32], F32)
    wr_hi = A.alloc([8, 32], BF16); wr_lo = A.alloc([8, 32], BF16)
    junk2 = A.alloc([1024], BF16)
    st2 = A.alloc([NT, 4], F32)
    lg = A.alloc([32], F32); top8 = A.alloc([8], F32); rt = A.alloc([8], F32)
    msk_t = A.alloc([32], F32); ex_t = A.alloc([32], F32)
    cum = A.alloc([32], F32); rk = A.alloc([32], F32); okm = A.alloc([32], F32); dstf = A.alloc([32], F32)
    m_bf = A.alloc([32], BF16); oh = A.alloc([32], F32)
    w8 = A.alloc([8], F32); i8 = A.alloc([8], U32); i8f = A.alloc([8], F32); dsel = A.alloc([4], F32)
    MEMSET("dve", cum, 0.0, ["cum"])
    DMA("sp", wr_f, wr_d.rearrange("(k p) n -> p k n", p=128), [], ["wr_f"], "const4", waitall=True)
    CP("act", wr_hi, wr_f, ["wr_f"], ["wr_hi"])
    TT("dve", wr_lo, wr_f, wr_hi, ALU.subtract, ["wr_f", "wr_hi"], ["wr_lo"])
    u_o0, k_o0 = load_unit(wsrc(wout_d, 0))
    u_o1, k_o1 = load_unit(wsrc(wout_d, 512))
    for n in range(NT):
        s = n % 2
        DMA("sp", xt2[:, s, :], x_d[n * 128:(n + 1) * 128, :], [], ["xt2%d" % s], "gxt2%d" % s)
        for hf, (uo, ko) in enumerate(((u_o0, k_o0), (u_o1, k_o1))):
            for kc in range(8):
                MM(bank(hf + 6 * (n % 2)), yT[:, kc, n * 128:(n + 1) * 128], uo[:, kc, :], kc == 0, kc == 7, [ko, "yT"], ["b%d" % (hf + 6 * (n % 2))])
            hc = slice(hf * 512, (hf + 1) * 512)
            TT("dve", x1[:, s, hc], bank(hf + 6 * (n % 2)), gt1_b[:, hc], ALU.mult, ["b%d" % (hf + 6 * (n % 2))], ["x1t%d_%d" % (s, hf), "x1_%d_%d" % (s, hf)])
            TT("dve", x1[:, s, hc], x1[:, s, hc], xt2[:, s, hc], ALU.add, ["x1t%d_%d" % (s, hf), "xt2%d" % s], ["x1_%d_%d" % (s, hf)])
        x1k = ["x1_%d_0" % s, "x1_%d_1" % s]
        DMA("sp", x1_d[n * 128:(n + 1) * 128, :], x1[:, s, :], x1k, ["x1d%d" % n], "gx1d%d" % s)
        ACTV(junk2, x1[:, s, :], AF.Square, x1k, ["junk2", "s2q%d" % s], accum=st2[:, n, 0:1])
        ACTV(st2[:, n, 1:2], st2[:, n, 0:1], AF.Sqrt, ["s2q%d" % s], ["s2r%d" % s], scale=1.0 / D, bias=1e-6)
        RECIP(st2[:, n, 2:3], st2[:, n, 1:2], ["s2r%d" % s], ["s2s%d" % s])
        STT("dve", h2[:, 0, :], x1[:, s, :], st2[:, n, 2:3], A2_b, ALU.mult, ALU.mult, x1k + ["s2s%d" % s], ["h2t"])
        TT("pool", h2[:, 0, :], h2[:, 0, :], B2_b, ALU.add, ["h2t"], ["h2"])
        CP("act", h2hi, h2[:, 0, :], ["h2"], ["h2hi"])
        TT("dve", h2lo, h2[:, 0, :], h2hi, ALU.subtract, ["h2", "h2hi"], ["h2lo"])
        for kc in range(8):
            TR(bank_bf(2)[:, kc * 128:(kc + 1) * 128], h2hi[:, kc * 128:(kc + 1) * 128], IDENT_B, ["h2hi", "cm_b"], ["b2"])
        for kc in range(8):
            TR(bank_bf(3)[:, kc * 128:(kc + 1) * 128], h2lo[:, kc * 128:(kc + 1) * 128], IDENT_B, ["h2lo", "cm_b"], ["b3"])
        CP("act", h2T[:, :, n * 128:(n + 1) * 128], bank_bf(2).rearrange("p (k t) -> p k t", k=8), ["b2"], ["h2T%d" % n])
        CP("dve", h2Tlo, bank_bf(3).rearrange("p (k t) -> p k t", k=8), ["b3"], ["h2Tlo"])
        for kc in range(8):
            MM(bank(4)[:, 0:32], h2T[:, kc, n * 128:(n + 1) * 128], wr_hi[:, kc, :], kc == 0, False, ["h2T%d" % n, "wr_hi"], ["b4"])
        for kc in range(8):
            MM(bank(4)[:, 0:32], h2Tlo[:, kc, :], wr_hi[:, kc, :], False, False, ["h2Tlo", "wr_hi"], ["b4"])
        for kc in range(8):
            MM(bank(4)[:, 0:32], h2T[:, kc, n * 128:(n + 1) * 128], wr_lo[:, kc, :], False, kc == 7, ["h2T%d" % n, "wr_lo"], ["b4"])
        TT("dve", lg, bank(4)[:, 0:32], brb, ALU.add, ["b4", "brb"], ["lg"])
        P.op("dve", lambda e: e.max(out=top8, in_=lg), ["lg"], ["top8"])
        TS("dve", msk_t, lg, top8[:, 3:4], ALU.is_ge, ["lg", "top8"], ["msk_t"])
        TS("dve", rt[:, 0:1], top8[:, 0:1], -1.0, ALU.mult, ["top8"], ["rt0"])
        ACTV(ex_t, lg, AF.Exp, ["lg", "rt0"], ["ex_t"], bias=rt[:, 0:1], scale=1.0)
        TT("dve", ex_t, ex_t, msk_t, ALU.mult, ["ex_t", "msk_t"], ["ex_t"])
        RED(rt[:, 1:2], ex_t, ["ex_t"], ["rt1"])
        RECIP(rt[:, 2:3], rt[:, 1:2], ["rt1"], ["rt2"])
        TS("dve", W_all[:, n, :], ex_t, rt[:, 2:3], ALU.mult, ["ex_t", "rt2"], ["W_all%d" % n])
        CP("dve", m_bf, msk_t, ["msk_t"], ["m_bf"])
        MM(bank(5)[:, 0:32], cm_b[:, 4, :], m_bf, True, True, ["m_bf", "cm_b"], ["b5"])
        MM(bank(5)[:, 32:64], ONES_B, m_bf, True, True, ["m_bf", "cm_b"], ["b5"])
        TT("dve", rk, bank(5)[:, 0:32], cum, ALU.add, ["b5", "cum"], ["rk"])
        TT("dve", cum, cum, bank(5)[:, 32:64], ALU.add, ["b5", "cum", "rk"], ["cum"])
        TS("dve", okm, rk, float(CAP), ALU.is_lt, ["rk"], ["okm"])
        TT("dve", okm, okm, msk_t, ALU.mult, ["okm", "msk_t"], ["okm"])
        TT("dve", W_all[:, n, :], W_all[:, n, :], okm, ALU.mult, ["W_all%d" % n, "okm"], ["W_all%d" % n])
        TT("dve", dstf, rk, moec[:, 32:64], ALU.add, ["rk", "moec"], ["dstf"])
        TS("dve", dstf, dstf, -float(TRASH), ALU.add, ["dstf"], ["dstf"])
        TT("dve", dstf, dstf, okm, ALU.mult, ["dstf", "okm"], ["dstf"])
        TS("dve", dstf, dstf, float(TRASH), ALU.add, ["dstf"], ["dstf"])
        P.op("dve", lambda e, n=n: e.max(out=w8, in_=W_all[:, n, :]), ["W_all%d" % n], ["w8"])
        P.op("dve", lambda e, n=n: e.max_index(out=i8, in_max=w8, in_values=W_all[:, n, :]), ["W_all%d" % n, "w8"], ["i8"])
        CP("dve", i8f, i8, ["i8"], ["i8f"])
        CP("dve", wsel[:, n, :], w8[:, 0:4], ["w8"], ["wsel%d" % n])
        for k in range(4):
            TS("dve", oh, moec[:, 0:32], i8f[:, k:k + 1], ALU.is_equal, ["moec", "i8f"], ["oh"])
            TT("dve", oh, oh, dstf, ALU.mult, ["oh", "dstf"], ["oh"])
            RED(dsel[:, k:k + 1], oh, ["oh"], ["dsel%d" % k])
        CP("dve", dest_i[:, n, :], dsel, ["dsel%d" % k for k in range(4)], ["dest%d" % n])
        for k in range(4):
            P.dma("pool", lambda e, n=n, k=k: e.indirect_dma_start(
                out=xs_d, out_offset=bass.IndirectOffsetOnAxis(ap=dest_i[:, n, k:k + 1], axis=0),
                in_=h2hi, in_offset=None),
                ["dest%d" % n, "h2hi"], ["xsb"], group="gsc%d" % k)
    assert A.top <= yT_start, (A.top, yT_start)
    if dbg:
        dump("d_W", W_all.rearrange("p a b -> p (a b)"), 512, ["W_all%d" % t for t in range(NT)], F32)
    P.barrier()
    if stop_after <= 5:
        return finish()
    A.release(m0)

    gsu = A.alloc([3, 3, 512], F32)
    gbuf = gsu[:, 0, :, :]; sbuf_ = gsu[:, 1, :, :]; ubuf = gsu[:, 2, :, :]
    NRT = CAP // 128
    xr = A.alloc([2, NRT, 1024], BF16)
    xsT2 = A.alloc([2, 8, CAP], BF16)
    b1T = A.alloc([N_EXP, 16], F32)
    b2p = A.alloc([1024], BF16)
    Wpad = A.alloc([128], BF16)
    WT = A.alloc([NT, 128], BF16)
    gs = A.alloc([8, CAP], BF16)
    actT = A.alloc([8, CAP], BF16)
    yst = A.alloc([6, 512], F32)
    accb = A.alloc([1024], F32)
    fin = gsu.rearrange("p a b c -> p (a b c)")[:, 0:2048].rearrange("p (a b) -> p a b", a=2)
    DMA("sp", b1T.rearrange("p a b -> p (a b)"), b1T_d, [], ["bias1T0"], "const5", waitall=True)
    TS("dve", b1T[:, :, 8:16], b1T[:, :, 8:16], 1.0, ALU.add, ["bias1T0"], ["bias1T"])
    MEMSET("dve", b2p, 0.0, ["b2p"])
    MEMSET("dve", Wpad, 0.0, ["Wpad"])
    MEMSET("dve", accb, 0.0, ["accb"])
    DMA("sp", ys_d[TRASH:TRASH + 128, :], accb, ["accb"], ["ysb"], "gysz")
    DMA("pool", b2p[0:32, :], b2_d, ["b2p"], ["b2p"], "const5b", waitall=True)
    for n in range(NT):
        CP("dve", Wpad[:, 0:32], W_all[:, n, :], ["Wpad"], ["Wpad"])
        TR(bank_bf(0)[:, 0:128], Wpad, IDENT_B, ["Wpad"], ["b0"])
        CP("act", WT[:, n, :], bank_bf(0)[:, 0:128], ["b0"], ["WT%d" % n])
    pcnt = 0
    ycnt = 0
    ecnt = 0
    NCB = CAP // 512

    def load_x(e):
        xs_ = e % 2
        DMA("sp", xr[:, xs_, :, :], xs_d[e * CAP:(e + 1) * CAP, :].rearrange("(r p) d -> p r d", p=128), ["xsb"], ["xr%d" % xs_], "gxr%d" % xs_)

    def load_w(e, u):
        if u < 4:
            return load_unit(w1_d[e, :, u * 512:(u + 1) * 512].rearrange("(k p) n -> p k n", p=128))
        return load_unit(w2_d[e, :, (u - 4) * 512:(u - 3) * 512].rearrange("(k p) n -> p k n", p=128))

    units = {}
    if n_exp_run > 0:
        load_x(0)
        for u in range(6):
            units[(0, u)] = load_w(0, u)
    def tr_task(e, r):
        nonlocal ecnt
        xs_ = e % 2
        tb_ = 4 + (ecnt % 2)
        ecnt += 1
        for kc in range(8):
            TR(bank_bf(tb_)[:, kc * 128:(kc + 1) * 128], xr[:, xs_, r, kc * 128:(kc + 1) * 128], IDENT_B, ["xr%d" % xs_, "cm_b"], ["b%d" % tb_])
        CP("act" if r % 2 == 0 else "dve", xsT2[:, xs_, :, r * 128:(r + 1) * 128], bank_bf(tb_).rearrange("p (k t) -> p k t", k=8),
           ["b%d" % tb_], ["xsT%d_%d" % (xs_, r)])

    if n_exp_run > 0:
        for r in range(NRT):
            tr_task(0, r)
    for e in range(n_exp_run):
        xs_ = e % 2
        xsT = xsT2[:, xs_, :, :]
        if e + 1 < n_exp_run:
            load_x(e + 1)
        for u in range(2):
            uu, uk = units[(e, u)]
            for cb in range(NCB):
                tcol = slice(cb * 512, (cb + 1) * 512)
                hk = ["xsT%d_%d" % (xs_, r) for r in range(cb * 4, cb * 4 + 4)]
                for mm_ in range(4):
                    m = u * 4 + mm_
                    pbk = (0, 1, 2, 3, 6, 7)[pcnt % 6]
                    pcnt += 1
                    s = pcnt % 3
                    for kc in range(8):
                        MM(bank(pbk), uu[:, kc, mm_ * 128:(mm_ + 1) * 128], xsT[:, kc, tcol], kc == 0, kc == 7, [uk] + hk, ["b%d" % pbk])
                    TS("dve", gbuf[:, s, :], bank(pbk), b1T[:, e, m:m + 1], ALU.add, ["b%d" % pbk, "bias1T"], ["gbuf%d" % s], s2=7.0, op1=ALU.min)
                    ACTV(sbuf_[:, s, :], gbuf[:, s, :], AF.Sigmoid, ["gbuf%d" % s], ["sbuf%d" % s], scale=1.702)
                    TT("dve", gs[:, m, tcol], gbuf[:, s, :], sbuf_[:, s, :], ALU.mult, ["gbuf%d" % s, "sbuf%d" % s], ["gs%d_%d" % (m, cb)])
            if e + 1 < n_exp_run:
                units[(e + 1, u)] = load_w(e + 1, u)
        for u in range(2, 4):
            uu, uk = units[(e, u)]
            for cb in range(NCB):
                tcol = slice(cb * 512, (cb + 1) * 512)
                hk = ["xsT%d_%d" % (xs_, r) for r in range(cb * 4, cb * 4 + 4)]
                for mm_ in range(4):
                    m = (u - 2) * 4 + mm_
                    pbk = (0, 1, 2, 3, 6, 7)[pcnt % 6]
                    pcnt += 1
                    s = pcnt % 3
                    for kc in range(8):
                        MM(bank(pbk), uu[:, kc, mm_ * 128:(mm_ + 1) * 128], xsT[:, kc, tcol], kc == 0, kc == 7, [uk] + hk, ["b%d" % pbk])
                    ACTV(ubuf[:, s, :], bank(pbk), AF.Identity, ["b%d" % pbk, "bias1T"], ["ubuf%d" % s], bias=b1T[:, e, 8 + m:9 + m], scale=1.0)
                    TS("dve", ubuf[:, s, :], ubuf[:, s, :], 8.0, ALU.min, ["ubuf%d" % s], ["ubuf%d" % s], s2=-6.0, op1=ALU.max)
                    TT("dve", actT[:, m, tcol], ubuf[:, s, :], gs[:, m, tcol], ALU.mult, ["ubuf%d" % s, "gs%d_%d" % (m, cb)], ["actT%d_%d" % (m, cb)])
            if e + 1 < n_exp_run:
                units[(e + 1, u)] = load_w(e + 1, u)
        for hf in range(2):
            uu, uk = units[(e, 4 + hf)]
            hc = slice(hf * 512, (hf + 1) * 512)
            for r in range(NRT):
                cb = r // 4
                ak = ["actT%d_%d" % (m, cb) for m in range(8)]
                pbk = (6, 7, 0, 1, 2, 3)[ycnt % 6]
                ys_ = ycnt % 6
                ycnt += 1
                for kc in range(8):
                    MM(bank(pbk), actT[:, kc, r * 128:(r + 1) * 128], uu[:, kc, :], kc == 0, kc == 7, [uk] + ak, ["b%d" % pbk])
                CP("act" if ys_ % 2 == 0 else "dve", yst[:, ys_, 0:512], bank(pbk), ["b%d" % pbk], ["yst%d" % ys_])
                r0 = e * CAP + r * 128
                DMA("sp", ys_d[r0:r0 + 128, hc], yst[:, ys_, 0:512], ["yst%d" % ys_], ["ysb"], "gys%d" % ys_)
                if e + 1 < n_exp_run and hf == 0:
                    tr_task(e + 1, r)
            if e + 1 < n_exp_run:
                units[(e + 1, 4 + hf)] = load_w(e + 1, 4 + hf)
    P.barrier()
    xrf = xr.rearrange("p a b c -> p (a b c)")
    gats = [xrf[:, 0:8192].bitcast(F32).rearrange("p (a b) -> p a b", a=4),
            xrf[:, 8192:16384].bitcast(F32).rearrange("p (a b) -> p a b", a=4)]
    for n in range(NT):
        s = n % 2
        gt_ = gats[s]
        DMA("sp", fin[:, s, :], x1_d[n * 128:(n + 1) * 128, :], ["x1d%d" % n], ["fin%d" % s], "gfin%d" % s)
        for k in range(4):
            P.dma("pool", lambda e, n=n, k=k, gt_=gt_: e.indirect_dma_start(
                out=gt_[:, k, :], out_offset=None, in_=ys_d,
                in_offset=bass.IndirectOffsetOnAxis(ap=dest_i[:, n, k:k + 1], axis=0)),
                ["ysb"], ["gat%d_%d" % (s, k)], group="ggat%d_%d" % (s, k))
        TS("dve", accb, gt_[:, 0, :], wsel[:, n, 0:1], ALU.mult, ["gat%d_0" % s], ["accb"])
        for k in range(1, 4):
            STT("dve", accb, gt_[:, k, :], wsel[:, n, k:k + 1], accb, ALU.mult, ALU.add, ["gat%d_%d" % (s, k), "accb"], ["accb"])
        for hf in range(2):
            hc = slice(hf * 512, (hf + 1) * 512)
            MM(bank(hf), WT[:, n, :], b2p[:, hc], True, True, ["WT%d" % n, "b2p"], ["b%d" % hf])
            TT("dve", accb[:, hc], accb[:, hc], bank(hf), ALU.add, ["accb", "b%d" % hf], ["accb"])
        TT("dve", accb, accb, gt2_b, ALU.mult, ["accb"], ["accb"])
        TT("dve", fin[:, s, :], fin[:, s, :], accb, ALU.add, ["fin%d" % s, "accb"], ["fin%d" % s])
        DMA("sp", out_d[n * 128:(n + 1) * 128, :], fin[:, s, :], ["fin%d" % s], ["outd%d" % n], "gout%d" % s, is_output=True)
    P.emit()
    es.close()
    return nc


def _host_consts():
    T = 2048
    t = np.arange(T)
    inv = 10000.0 ** (-np.arange(0, 32, 2, dtype=np.float64) / 32)
    ar = (t // 64)[:, None] * inv
    ac = (t % 64)[:, None] * inv
    C = np.concatenate([np.cos(ar), np.cos(ar), np.cos(ac), np.cos(ac)], axis=1)
    S = np.concatenate([-np.sin(ar), np.sin(ar), -np.sin(ac), np.sin(ac)], axis=1)
    ropeC = np.ascontiguousarray(np.tile(C.reshape(16, 128, 1, 64), (1, 1, 10, 1)).reshape(16, 128, 640).astype(np.float32))
    ropeS = np.ascontiguousarray(np.tile(S.reshape(16, 128, 1, 64), (1, 1, 10, 1)).reshape(16, 128, 640).astype(np.float32))
    p = np.arange(128)[:, None]; f = np.arange(128)[None, :]
    cm = np.stack([(p == f), (p <= f), (p > f), (p >= f), (p < f), np.ones((128, 128), bool)], axis=1).astype(np.float32)
    return ropeC, ropeS, np.ascontiguousarray(cm.reshape(128, 6 * 128))


_NC_CACHE = {}


def _lay(v, k):
    return np.ascontiguousarray(v.reshape(k, 128).T)


def _prep(x, c, ctx, c_ctx, w_mod, b_mod, norm1, norm2, w_in, q_norm, k_norm, attn_sink,
          w_alpha_f, b_alpha_f, w_alpha_b, b_alpha_b, gla_norm, w_branch_attn, w_branch_gla,
          w_out, w_router, b_router, w_exp_in, b_exp_in, w_exp_out, b_exp_out):
    f = lambda a: np.ascontiguousarray(np.asarray(a, dtype=np.float32))
    x = f(x); c = f(c); ctx = f(ctx); c_ctx = f(c_ctx)
    ropeC, ropeS, cm = _host_consts()
    rep = lambda v: np.ascontiguousarray(np.broadcast_to(f(v).reshape(1, -1), (128, f(v).size)))
    bm = f(b_mod)[0]
    shared = {
        "w_mod": f(w_mod)[0], "b_modT": _lay(bm[0:2048], 16), "b_mod_b": rep(bm[2048:6144]),
        "norm1T": _lay(f(norm1)[0], 8), "norm2_b": rep(f(norm2)[0]),
        "w_in": f(w_in)[0], "qk_gain_b": rep(np.concatenate([f(q_norm)[0], f(k_norm)[0]])),
        "sink_b": rep(f(attn_sink)[0]),
        "w_alpha_f_aug": np.ascontiguousarray(np.concatenate([f(w_alpha_f)[0], f(b_alpha_f)[0][None, :], np.zeros((15, 256), np.float32)], axis=0)),
        "w_alpha_b_aug": np.ascontiguousarray(np.concatenate([f(w_alpha_b)[0], f(b_alpha_b)[0][None, :], np.zeros((15, 256), np.float32)], axis=0)),
        "gla_norm_b": rep(f(gla_norm)[0]),
        "w_branch_attn": f(w_branch_attn)[0], "w_branch_gla": f(w_branch_gla)[0], "w_out": f(w_out)[0],
        "w_router": f(w_router)[0], "b_router_b": rep(f(b_router)[0]),
        "w_exp_in": f(w_exp_in)[0],
        "bias1T": np.ascontiguousarray(f(b_exp_in)[0].reshape(N_EXP, 16, 128).transpose(2, 0, 1).reshape(128, N_EXP * 16)),
        "w_exp_out": f(w_exp_out)[0], "b_exp_out": f(b_exp_out)[0],
        "ropeC": ropeC, "ropeS": ropeS, "cmask": cm,
        "moe_c": np.ascontiguousarray(np.broadcast_to(np.concatenate([np.arange(32), np.arange(32) * CAP]).astype(np.float32)[None, :], (128, 64))),
        "hm": np.ascontiguousarray(np.stack([(np.arange(128) < 64), (np.arange(128) >= 64)], axis=1).astype(np.float32)),
    }
    in_maps = []
    for b in range(8):
        m = dict(shared)
        m["x"] = x[b]; m["ctx"] = ctx[b]
        m["cvec"] = np.ascontiguousarray(np.stack([_lay(c[b], 8), _lay(c_ctx, 8)], axis=2).reshape(128, 16))
        in_maps.append(m)
    return in_maps


def kernel(**inputs):
    if "nc" not in _NC_CACHE:
        _NC_CACHE["nc"] = build()
    nc = _NC_CACHE["nc"]
    in_maps = _prep(**inputs)
    res = run_bass_kernel_spmd(nc, in_maps, core_ids=list(range(8)))
    return np.stack([np.asarray(res.results[b]["out"], dtype=np.float32) for b in range(8)], axis=0)
```
